# Optimizing a Trainium2 kernel written in Bass

```python
import jax, jax.numpy as jnp
from jax import lax
import numpy as np

D_MODEL = 2048
BATCH = 2
SEQ = 8192
DEPTH = 2

N_A_LAYERS = DEPTH // 2
N_B_LAYERS = DEPTH - N_A_LAYERS
N_DENSE = (DEPTH + 1) // 2
N_MOE = DEPTH // 2

HG_HEADS = 16
HG_KDIM = 128
HG_VDIM = D_MODEL // HG_HEADS
HG_FDIM = HG_HEADS * HG_KDIM
HG_CHUNK = 64

MLA_HEADS = 16
Q_LORA = 512
KV_LORA = 512
QK_NOPE = 128
QK_ROPE = 64
V_HEAD = 128
MLA_SCALE = (QK_NOPE + QK_ROPE) ** -0.5
ROPE_THETA = 10000.0
Q_BLOCK = 128

D_FF = 5632
N_EXPERTS = 8
TOP_K = 2
MOE_ROWS = 256

EPS = 1e-6

kernel_name = 'hybrid_hgrn2_mla_moe_yoco'


def rms_norm(x, g):
    xf = x.astype(jnp.float32)
    y = xf * lax.rsqrt(jnp.mean(xf * xf, axis=-1, keepdims=True) + EPS)
    return (y * g.astype(jnp.float32)).astype(x.dtype)


def modulation(sc, w, b, n):
    m = sc @ w + b
    return [t[:, None, :] for t in jnp.split(m, n, axis=-1)]


def rope_tables(positions):
    inv = 1.0 / (ROPE_THETA ** (jnp.arange(0, QK_ROPE, 2, dtype=jnp.float32) / QK_ROPE))
    ang = positions.astype(jnp.float32)[..., None] * inv
    return jnp.cos(ang), jnp.sin(ang)


def apply_rope(x, cos, sin):
    x1, x2 = jnp.split(x, 2, axis=-1)
    return jnp.concatenate([x1 * cos - x2 * sin, x2 * cos + x1 * sin], axis=-1).astype(x.dtype)


def hgrn2_mixer(u, w_in, lower_bound, out_norm_g, w_out):
    B, S, _ = u.shape
    n_chunks = S // HG_CHUNK
    proj = u @ w_in
    q, f_logit, i, g = jnp.split(proj, [HG_FDIM, 2 * HG_FDIM, 2 * HG_FDIM + D_MODEL], axis=-1)
    f = lower_bound + (1.0 - lower_bound) * jax.nn.sigmoid(f_logit.astype(jnp.float32))
    k = 1.0 - f
    log_f = jnp.log(f)

    def chunks(t, d):
        return t.astype(jnp.float32).reshape(B, n_chunks, HG_CHUNK, HG_HEADS, d).transpose(1, 0, 3, 2, 4)

    xs = (chunks(q, HG_KDIM), chunks(k, HG_KDIM), chunks(log_f, HG_KDIM), chunks(i, HG_VDIM))
    causal = jnp.tril(jnp.ones((HG_CHUNK, HG_CHUNK), dtype=bool))[:, :, None]

    def step(state, inp):
        qc, kc, lfc, vc = inp
        b = jnp.cumsum(lfc, axis=2)
        o_inter = jnp.einsum('bhtk,bhkv->bhtv', qc * jnp.exp(b), state)
        rel = b[:, :, :, None, :] - b[:, :, None, :, :]
        decay = jnp.exp(jnp.where(causal, rel, -jnp.inf))
        scores = jnp.einsum('bhtk,bhsk,bhtsk->bhts', qc, kc, decay)
        o_intra = jnp.einsum('bhts,bhsv->bhtv', scores, vc)
        b_last = b[:, :, -1:, :]
        state = (jnp.exp(b_last[:, :, 0, :])[..., None] * state
                 + jnp.einsum('bhsk,bhsv->bhkv', kc * jnp.exp(b_last - b), vc))
        return state, o_inter + o_intra

    s0 = jnp.zeros((B, HG_HEADS, HG_KDIM, HG_VDIM), jnp.float32)
    _, o = lax.scan(step, s0, xs)
    o = o.transpose(1, 0, 3, 2, 4).reshape(B, S, HG_HEADS * HG_VDIM).astype(u.dtype)
    o = rms_norm(o, out_norm_g) * jax.nn.silu(g)
    return o @ w_out


def mla_shared_kv(xs, w_kv_a, kv_norm_g, w_kv_b, cos, sin):
    B, S, _ = xs.shape
    c_kv, k_rope = jnp.split(xs @ w_kv_a, [KV_LORA], axis=-1)
    kv = (rms_norm(c_kv, kv_norm_g) @ w_kv_b).reshape(B, S, MLA_HEADS, QK_NOPE + V_HEAD)
    k_nope, v = jnp.split(kv, [QK_NOPE], axis=-1)
    k_rope = apply_rope(k_rope, cos, sin)
    return k_nope, k_rope, v


def mla_attention(u, k_nope, k_rope, v, w_q_a, q_norm_g, w_q_b, w_o, cos, sin):
    B, S, _ = u.shape
    q = (rms_norm(u @ w_q_a, q_norm_g) @ w_q_b).reshape(B, S, MLA_HEADS, QK_NOPE + QK_ROPE)
    q_nope, q_rope = jnp.split(q, [QK_NOPE], axis=-1)
    q_rope = apply_rope(q_rope, cos[:, :, None, :], sin[:, :, None, :])
    n_blk = S // Q_BLOCK

    def blocks(t):
        return t.reshape(B, n_blk, Q_BLOCK, *t.shape[2:]).swapaxes(0, 1)

    key_pos = jnp.arange(S)

    def attend(inp):
        qn, qr, blk = inp
        s = (jnp.einsum('bqhd,bkhd->bhqk', qn, k_nope)
             + jnp.einsum('bqhr,bkr->bhqk', qr, k_rope)).astype(jnp.float32) * MLA_SCALE
        q_pos = blk * Q_BLOCK + jnp.arange(Q_BLOCK)
        s = jnp.where(key_pos[None, :] <= q_pos[:, None], s, -jnp.inf)
        p = jax.nn.softmax(s, axis=-1).astype(v.dtype)
        return jnp.einsum('bhqk,bkhd->bqhd', p, v)

    o = lax.map(attend, (blocks(q_nope), blocks(q_rope), jnp.arange(n_blk)))
    o = o.swapaxes(0, 1).reshape(B, S, MLA_HEADS * V_HEAD)
    return o @ w_o


def swiglu(h, w_gu, w_down):
    gt, up = jnp.split(h @ w_gu, 2, axis=-1)
    return (jax.nn.silu(gt) * up) @ w_down


def moe_swiglu(h, w_router, w_gu, w_down):
    B, S, D = h.shape
    n_tok = B * S
    hf = h.reshape(n_tok, D)
    logits = (hf @ w_router).astype(jnp.float32)
    top_logit, top_idx = lax.top_k(logits, TOP_K)
    top_w = jax.nn.softmax(top_logit, axis=-1)
    n_asg = n_tok * TOP_K
    asg_e = top_idx.reshape(n_asg)
    asg_t = jnp.arange(n_asg, dtype=jnp.int32) // TOP_K
    asg_w = top_w.reshape(n_asg)
    order = jnp.argsort(asg_e)
    sorted_e = asg_e[order]
    counts = jnp.bincount(asg_e, length=N_EXPERTS)
    padded = (counts + MOE_ROWS - 1) // MOE_ROWS * MOE_ROWS
    start = jnp.cumsum(counts) - counts
    pend = jnp.cumsum(padded)
    pstart = pend - padded
    dest = pstart[sorted_e] + (jnp.arange(n_asg) - start[sorted_e])
    n_blocks = (n_asg + N_EXPERTS * (MOE_ROWS - 1)) // MOE_ROWS
    n_rows = n_blocks * MOE_ROWS
    row_tok = jnp.zeros((n_rows,), jnp.int32).at[dest].set(asg_t[order])
    row_w = jnp.zeros((n_rows,), jnp.float32).at[dest].set(asg_w[order])
    blk_e = jnp.minimum(jnp.searchsorted(pend, jnp.arange(n_blocks) * MOE_ROWS, side='right'),
                        N_EXPERTS - 1)

    def expert_block(inp):
        tok, e = inp
        return swiglu(hf[tok], w_gu[e], w_down[e])

    y = lax.map(expert_block, (row_tok.reshape(n_blocks, MOE_ROWS), blk_e))
    y = y.reshape(n_rows, D) * row_w[:, None].astype(h.dtype)
    out = jnp.zeros_like(hf).at[row_tok].add(y)
    return out.reshape(B, S, D)


def setup_inputs(seed: int = 0) -> dict:
    key = jax.random.key(seed)
    ks = jax.random.split(key, 26)
    f32 = jnp.float32
    D = D_MODEL

    def nrm(k, shape, fan_in, gain=1.0):
        return jax.random.normal(k, shape, f32) * (gain * fan_in ** -0.5)

    def gains(k, shape):
        return 1.0 + 0.02 * jax.random.normal(k, shape, f32)

    x = jax.random.normal(ks[0], (BATCH, SEQ, D), f32)
    c = jax.random.normal(ks[1], (BATCH, D), f32)
    positions = (jax.random.randint(ks[2], (BATCH, 1), 0, 1024)
                 + jnp.arange(SEQ)[None, :]).astype(jnp.int32)
    return {
        'x': x,
        'c': c,
        'positions': positions,
        'ada_w': nrm(ks[3], (DEPTH, 2, D, 3 * D), D, 0.5),
        'ada_b': 0.02 * jax.random.normal(ks[4], (DEPTH, 2, 3 * D), f32),
        'norm_g': gains(ks[5], (DEPTH, 2, 2, D)),
        'hg_w_in': nrm(ks[6], (N_A_LAYERS, D, 2 * HG_FDIM + 2 * D), D),
        'hg_lb_logits': 0.5 * jax.random.normal(ks[7], (N_A_LAYERS + 1, HG_FDIM), f32),
        'hg_out_norm_g': gains(ks[8], (N_A_LAYERS, D)),
        'hg_w_out': nrm(ks[9], (N_A_LAYERS, D, D), D),
        'kv_src_norm_g': gains(ks[10], (D,)),
        'kv_src_ada_w': nrm(ks[11], (D, 2 * D), D, 0.5),
        'kv_src_ada_b': 0.02 * jax.random.normal(ks[12], (2 * D,), f32),
        'mla_w_kv_a': nrm(ks[13], (D, KV_LORA + QK_ROPE), D),
        'mla_kv_norm_g': gains(ks[14], (KV_LORA,)),
        'mla_w_kv_b': nrm(ks[15], (KV_LORA, MLA_HEADS * (QK_NOPE + V_HEAD)), KV_LORA),
        'mla_w_q_a': nrm(ks[16], (N_B_LAYERS, D, Q_LORA), D),
        'mla_q_norm_g': gains(ks[17], (N_B_LAYERS, Q_LORA)),
        'mla_w_q_b': nrm(ks[18], (N_B_LAYERS, Q_LORA, MLA_HEADS * (QK_NOPE + QK_ROPE)), Q_LORA),
        'mla_w_o': nrm(ks[19], (N_B_LAYERS, MLA_HEADS * V_HEAD, D), MLA_HEADS * V_HEAD),
        'ffn_w_gu': nrm(ks[20], (N_DENSE, D, 2 * D_FF), D),
        'ffn_w_down': nrm(ks[21], (N_DENSE, D_FF, D), D_FF),
        'moe_w_router': nrm(ks[22], (N_MOE, D, N_EXPERTS), D),
        'moe_w_gu': nrm(ks[23], (N_MOE, N_EXPERTS, D, 2 * D_FF), D),
        'moe_w_down': nrm(ks[24], (N_MOE, N_EXPERTS, D_FF, D), D_FF),
    }


def reference(x, c, positions, ada_w, ada_b, norm_g, hg_w_in, hg_lb_logits, hg_out_norm_g,
              hg_w_out, kv_src_norm_g, kv_src_ada_w, kv_src_ada_b, mla_w_kv_a, mla_kv_norm_g,
              mla_w_kv_b, mla_w_q_a, mla_q_norm_g, mla_w_q_b, mla_w_o, ffn_w_gu, ffn_w_down,
              moe_w_router, moe_w_gu, moe_w_down):
    lb_all = jnp.cumsum(jax.nn.softmax(hg_lb_logits.astype(jnp.float32), axis=0), axis=0)
    cos, sin = rope_tables(positions)
    sc = jax.nn.silu(c)
    h = x
    k_nope = k_rope = v = None
    for layer in range(DEPTH):
        if layer == N_A_LAYERS:
            shift, scale = modulation(sc, kv_src_ada_w, kv_src_ada_b, 2)
            xs = rms_norm(h, kv_src_norm_g) * (1.0 + scale) + shift
            k_nope, k_rope, v = mla_shared_kv(xs, mla_w_kv_a, mla_kv_norm_g, mla_w_kv_b, cos, sin)

        shift, scale, gate = modulation(sc, ada_w[layer, 0], ada_b[layer, 0], 3)
        u = rms_norm(h, norm_g[layer, 0, 0]) * (1.0 + scale) + shift
        if layer < N_A_LAYERS:
            a = layer
            y = hgrn2_mixer(u, hg_w_in[a], lb_all[a], hg_out_norm_g[a], hg_w_out[a])
        else:
            bl = layer - N_A_LAYERS
            y = mla_attention(u, k_nope, k_rope, v, mla_w_q_a[bl], mla_q_norm_g[bl],
                              mla_w_q_b[bl], mla_w_o[bl], cos, sin)
        h = h + gate * rms_norm(y, norm_g[layer, 0, 1])

        shift, scale, gate = modulation(sc, ada_w[layer, 1], ada_b[layer, 1], 3)
        u = rms_norm(h, norm_g[layer, 1, 0]) * (1.0 + scale) + shift
        j = layer // 2
        if layer % 2 == 0:
            y = swiglu(u, ffn_w_gu[j], ffn_w_down[j])
        else:
            y = moe_swiglu(u, moe_w_router[j], moe_w_gu[j], moe_w_down[j])
        h = h + gate * rms_norm(y, norm_g[layer, 1, 1])
    return h
```

```python
import numpy as np
from contextlib import ExitStack
import concourse.bass as bass
import concourse.mybir as mybir

F32 = mybir.dt.float32
BF16 = mybir.dt.bfloat16
I32 = mybir.dt.int32
AF = mybir.ActivationFunctionType
ALU = mybir.AluOpType

ENGS = ("pe", "act", "dve", "pool", "sp")
NDMASEM = 12


class Unit:
    __slots__ = ("name", "writer", "readers", "dreaders")

    def __init__(self, name):
        self.name = name
        self.writer = None
        self.readers = {}
        self.dreaders = []


class V:
    __slots__ = ("u", "ap")

    def __init__(self, u, ap):
        self.u = u
        self.ap = ap

    def __getitem__(self, idx):
        return V(self.u, self.ap[idx])


class Op:
    __slots__ = ("eng", "fn", "waits", "is_dma", "signal", "idx", "dsem", "dval", "cnt", "qi")

    def __init__(self, eng, fn, is_dma):
        self.eng = eng
        self.fn = fn
        self.is_dma = is_dma
        self.waits = []
        self.signal = False
        self.cnt = None


class K:
    def __init__(self, name="k"):
        self.nc = bass.Bass("TRN2", target_bir_lowering=False)
        self.es = ExitStack()
        self.ops = {e: [] for e in ENGS}
        self.ndma = {e: 0 for e in ENGS}
        self.dma_ops = {e: [] for e in ENGS}
        self.n = 0
        self.out_dmas = []

    def dram(self, name, shape, dt, kind):
        return self.nc.dram_tensor(name, list(shape), dt, kind=kind).ap()

    def sb(self, name, shape, dt=F32, units=1):
        t = self.es.enter_context(self.nc.sbuf_tensor("s_" + name, list(shape), dt))
        if units == 1:
            return V(Unit(name), t[:])
        return [V(Unit(f"{name}{j}"), t[:, j]) for j in range(units)]

    def ps(self, name, shape=(128, 512), dt=F32, units=1):
        t = self.es.enter_context(self.nc.psum_tensor("p_" + name, list(shape), dt))
        if units == 1:
            return V(Unit(name), t[:])
        return [V(Unit(f"{name}{j}"), t[:, j]) for j in range(units)]

    def _rec(self, eng, fn, reads, writes, is_dma=False):
        op = Op(eng, fn, is_dma)
        op.idx = len(self.ops[eng])
        deps = []
        for v in reads:
            u = v.u
            if u.writer is not None:
                deps.append((u.writer, "raw"))
        for v in writes:
            u = v.u
            if u.writer is not None:
                deps.append((u.writer, "waw"))
            for r in list(u.readers.values()) + u.dreaders:
                deps.append((r, "war"))
        seen = set()
        for p, kind in deps:
            if p is op or id(p) in seen:
                continue
            if (not p.is_dma) and p.eng == eng and not is_dma:
                if eng == "pe" or kind == "war":
                    continue
            seen.add(id(p))
            op.waits.append(p)
            p.signal = True
        for v in reads:
            if is_dma:
                v.u.dreaders.append(op)
            else:
                v.u.readers[eng] = op
        for v in writes:
            v.u.writer = op
            v.u.readers = {}
            v.u.dreaders = []
        if is_dma:
            op.qi = self.ndma[eng]
            self.ndma[eng] += 1
            self.dma_ops[eng].append(op)
            op.signal = True
        self.ops[eng].append(op)
        return op

    def eng_obj(self, e):
        nc = self.nc
        return {"pe": nc.tensor, "act": nc.scalar, "dve": nc.vector, "pool": nc.gpsimd, "sp": nc.sync}[e]

    def mm(self, out, lhsT, rhs, start=True, stop=True):
        return self._rec("pe", lambda e: e.matmul(out.ap, lhsT.ap, rhs.ap, start=start, stop=stop),
                         [lhsT, rhs] + ([] if start else [out]), [out])

    def transpose(self, out, in_, ident):
        return self._rec("pe", lambda e: e.transpose(out.ap, in_.ap, ident.ap), [in_, ident], [out])

    def act(self, out, in_, func, bias=None, scale=None, extra_reads=()):
        kw = {}
        rd = [in_] + list(extra_reads)
        if bias is not None:
            if isinstance(bias, V):
                kw["bias"] = bias.ap
                rd.append(bias)
            else:
                kw["bias"] = bias
        if scale is not None:
            if isinstance(scale, V):
                kw["scale"] = scale.ap
                rd.append(scale)
            else:
                kw["scale"] = scale
        return self._rec("act", lambda e: e.activation(out.ap, in_.ap, func, **kw), rd, [out])

    def tt(self, out, in0, in1, op, eng="dve"):
        return self._rec(eng, lambda e: e.tensor_tensor(out.ap, in0.ap, in1.ap, op), [in0, in1], [out])

    def ts(self, out, in0, s1, op0, s2=None, op1=None, eng="dve"):
        rd = [in0]
        a1 = s1
        a2 = s2
        if isinstance(s1, V):
            rd.append(s1)
            a1 = s1.ap
        if isinstance(s2, V):
            rd.append(s2)
            a2 = s2.ap
        if op1 is None:
            return self._rec(eng, lambda e: e.tensor_scalar(out.ap, in0.ap, a1, None, op0), rd, [out])
        return self._rec(eng, lambda e: e.tensor_scalar(out.ap, in0.ap, a1, a2, op0, op1), rd, [out])

    def stt(self, out, in0, scalar, in1, op0, op1):
        rd = [in0, in1]
        a = scalar
        if isinstance(scalar, V):
            rd.append(scalar)
            a = scalar.ap
        return self._rec("dve", lambda e: e.scalar_tensor_tensor(out.ap, in0.ap, a, in1.ap, op0, op1), rd, [out])

    def copy(self, out, in_, eng="dve"):
        if eng == "act":
            return self._rec("act", lambda e: e.copy(out.ap, in_.ap), [in_], [out])
        return self._rec(eng, lambda e: e.tensor_copy(out.ap, in_.ap), [in_], [out])

    def memset(self, out, val, eng="dve"):
        return self._rec(eng, lambda e: e.memset(out.ap, val), [], [out])

    def recip(self, out, in_):
        return self._rec("dve", lambda e: e.reciprocal(out.ap, in_.ap), [in_], [out])

    def generic(self, eng, fn, reads, writes):
        return self._rec(eng, fn, reads, writes)

    def dma(self, out, in_, q="sp", is_output=False):
        rd = [in_] if isinstance(in_, V) else []
        wr = [out] if isinstance(out, V) else []
        oa = out.ap if isinstance(out, V) else out
        ia = in_.ap if isinstance(in_, V) else in_
        op = self._rec(q, lambda e: e.dma_start(out=oa, in_=ia), rd, wr, is_dma=True)
        if is_output:
            self.out_dmas.append(op)
        return op

    def finish(self):
        nc = self.nc
        es = self.es
        csem = {e: es.enter_context(nc.semaphore(f"c_{e}")) for e in ENGS}
        dsem = {e: [es.enter_context(nc.semaphore(f"d_{e}{i}")) for i in range(NDMASEM)]
                for e in ENGS if self.ndma[e] > 0}
        for e in ENGS:
            c = 0
            for op in self.ops[e]:
                if op.is_dma:
                    op.dsem = dsem[e][op.qi % NDMASEM]
                    op.dval = 16 * (op.qi // NDMASEM + 1)
                elif op.signal:
                    c += 1
                    op.cnt = c
        final_waits = list(self.out_dmas)
        block = es.enter_context(nc.Block())

        def emit(e):
            eng = self.eng_obj(e)
            seen_c = {x: 0 for x in ENGS}
            seen_d = {}
            oplist = self.ops[e]

            def wait_on(p):
                if p.is_dma:
                    key = id(p.dsem)
                    if seen_d.get(key, 0) >= p.dval:
                        return
                    eng.wait_ge(p.dsem, p.dval)
                    seen_d[key] = p.dval
                else:
                    if seen_c[p.eng] >= p.cnt:
                        return
                    eng.wait_ge(csem[p.eng], p.cnt)
                    seen_c[p.eng] = p.cnt

            for op in oplist:
                for p in op.waits:
                    wait_on(p)
                if op.is_dma and op.qi >= NDMASEM:
                    wait_on(self.dma_ops[e][op.qi - NDMASEM])
                ins = op.fn(eng)
                if op.is_dma:
                    ins.then_inc(op.dsem, 16)
                elif op.signal:
                    ins.then_inc(csem[e], 1)
            if e == "sp":
                for p in final_waits:
                    wait_on(p)

        block.sync(lambda s: emit("sp"))
        block.tensor(lambda s: emit("pe"))
        block.scalar(lambda s: emit("act"))
        block.vector(lambda s: emit("dve"))
        block.gpsimd(lambda s: emit("pool"))
        es.close()
        return nc

D = 2048
DC = 16
EPS = 1e-6


def chunked(ap, p=128):
    return ap.rearrange("(c p) t -> p c t", p=p)


class Ctx:
    def __init__(self, k, ident_dram=None):
        self.k = k
        self.ones = k.sb("ones_bf", [128, 128], BF16)
        k.memset(self.ones, 1.0)
        self.eps = k.sb("eps_t", [128, 1], F32)
        k.memset(self.eps, EPS)
        if ident_dram is not None:
            self.ident = k.sb("ident_bf", [128, 128], BF16)
            k.dma(self.ident, ident_dram, q="pool")
        self.pi = 0

    def rstd(self, k, x_units, TT, sq_units, ps, out, nfeat):
        n = len(x_units)
        for c in range(n):
            k.act(sq_units[c][:, :TT], x_units[c][:, :TT], AF.Square)
        for c in range(n):
            k.mm(ps[:, :TT], self.ones, sq_units[c][:, :TT], start=(c == 0), stop=(c == n - 1))
        k.act(out[:, :TT], ps[:, :TT], AF.Sqrt, bias=self.eps, scale=1.0 / nfeat)
        k.recip(out[:, :TT], out[:, :TT])


def mod_coefs(k, name, mods, g, shift_c, scale_c, n=DC):
    A = k.sb(name + "_A", [128, n], F32)
    k.stt(A, mods[:, scale_c:scale_c + n], 1.0, g, ALU.add, ALU.mult)
    return A, mods[:, shift_c:shift_c + n]


def gate_coefs(k, name, mods, g, gate_c, n=DC):
    G = k.sb(name + "_G", [128, n], F32)
    k.tt(G, mods[:, gate_c:gate_c + n], g, ALU.mult)
    return G


def norm_mod_apply(k, x_units, rstd, A, B, tmp_units, out_units, TT, out2_units=None):
    n = len(x_units)
    for c in range(n):
        k.tt(tmp_units[c][:, :TT], x_units[c][:, :TT], rstd[:, :TT], ALU.mult)
        k.act(out_units[c][:, :TT], tmp_units[c][:, :TT], AF.Identity, bias=B[:, c:c + 1], scale=A[:, c:c + 1])
        if out2_units is not None:
            k.ts(out2_units[c][:, :TT], tmp_units[c][:, :TT], A[:, c:c + 1], ALU.mult, B[:, c:c + 1], ALU.add)


class Slabs:
    def __init__(self, k, name, KC, nbuf, width=128):
        self.k = k
        self.bufs = [k.sb(f"{name}{i}", [128, KC, width], BF16) for i in range(nbuf)]
        self.i = 0
        self.KC = KC

    def load(self, W, c0, width=128, kc=None):
        b = self.bufs[self.i % len(self.bufs)]
        self.i += 1
        kc = kc or self.KC
        self.k.dma(b[:, :kc, :width], W.rearrange("(kc p) n -> p kc n", p=128)[:, :, c0:c0 + width], q="pool")
        return b


def build_L0(NCH=28):
    k = K()
    cT = k.dram("cT", [128, DC, 2], F32, "ExternalInput")
    W = k.dram("W", [D, NCH * 128], F32, "ExternalInput")
    bias = k.dram("bias", [128, NCH], F32, "ExternalInput")
    out = k.dram("out", [128, NCH, 2], F32, "ExternalOutput")
    c_sb = k.sb("c_sb", [128, DC, 2], F32)
    sc = k.sb("sc", [128, DC, 2], F32)
    b_sb = k.sb("b_sb", [128, NCH], F32)
    o_sb = k.sb("o_sb", [128, NCH, 2], F32)
    slabs = [k.sb(f"sl{i}", [128, DC, 128], F32) for i in range(4)]
    pss = [k.ps(f"ps{i}") for i in range(4)]
    k.dma(c_sb, cT)
    k.dma(b_sb, bias)
    k.act(sc, c_sb, AF.Silu)
    Wv = W.rearrange("(kc p) n -> p kc n", p=128)
    for j in range(NCH):
        sl = slabs[j % 4]
        k.dma(sl, Wv[:, :, j * 128:(j + 1) * 128], q=("sp" if j % 2 == 0 else "act"))
        ps = pss[j % 4]
        for kc in range(DC):
            k.mm(ps[:, 0:2], sl[:, kc, :], sc[:, kc, :], start=(kc == 0), stop=(kc == DC - 1))
        k.ts(o_sb[:, j, :], ps[:, 0:2], b_sb[:, j:j + 1], ALU.add)
    k.dma(out, o_sb, is_output=True)
    return k.finish()


def build_L1(Tc, TT=512):
    k = K()
    xT = k.dram("xT", [D, Tc], F32, "ExternalInput")
    mods = k.dram("mods", [128, 224], F32, "ExternalInput")
    gains = k.dram("gains", [128, DC], F32, "ExternalInput")
    uT = k.dram("uT", [D, Tc], BF16, "ExternalOutput")
    cx = Ctx(k)
    m_sb = k.sb("m_sb", [128, 224], F32)
    g_sb = k.sb("g_sb", [128, DC], F32)
    k.dma(m_sb, mods)
    k.dma(g_sb, gains)
    A, B = mod_coefs(k, "n0", m_sb, g_sb, 0, 16)
    NB = 2
    xs = [k.sb(f"x{b}", [128, DC, TT], F32, units=DC) for b in range(NB)]
    sq = k.sb("sq", [128, DC, TT], BF16, units=DC)
    tmp = k.sb("tmp", [128, DC, TT], F32, units=DC)
    us = [k.sb(f"u{b}", [128, DC, TT], BF16, units=DC) for b in range(NB)]
    rs = [k.sb(f"rs{b}", [128, TT], F32) for b in range(NB)]
    pss = [k.ps(f"ps{i}") for i in range(2)]
    xv = chunked(xT)
    uv = chunked(uT)
    for t in range(Tc // TT):
        b = t % NB
        for c in range(DC):
            k.dma(xs[b][c], xv[:, c, t * TT:(t + 1) * TT], q=("sp" if c % 2 == 0 else "act"))
        cx.rstd(k, xs[b], TT, sq, pss[b], rs[b], D)
        norm_mod_apply(k, xs[b], rs[b], A, B, tmp, us[b], TT)
        for c in range(DC):
            k.dma(uv[:, c, t * TT:(t + 1) * TT], us[b][c], q="sp", is_output=True)
    return k.finish()


def build_L2(B, S, TT=512):
    k = K()
    T = B * S
    uT = k.dram("uT", [D, T], BF16, "ExternalInput")
    Wc = k.dram("Wc", [D, 1024], F32, "ExternalInput")
    lbl = k.dram("lbl", [128, 2, 2], F32, "ExternalInput")
    identd = k.dram("ident", [128, 128], BF16, "ExternalInput")
    trid = k.dram("tri", [64, 64], F32, "ExternalInput")
    rmaskd = k.dram("rmask", [128, TT], F32, "ExternalInput")
    oT = k.dram("oT", [256, T], BF16, "ExternalOutput")
    sgT = k.dram("sgT", [256, T], BF16, "ExternalOutput")
    cx = Ctx(k, identd)
    tri = k.sb("tri", [64, 64], F32)
    k.dma(tri, trid)
    rmask = k.sb("rmask", [128, TT], F32)
    k.dma(rmask, rmaskd)
    w_sb = k.sb("w_sb", [128, DC, 1024], BF16)
    for kc in range(DC):
        k.dma(w_sb[:, kc, :], Wc[kc * 128:(kc + 1) * 128, :], q="pool")
    lb_in = k.sb("lb_in", [128, 2, 2], F32)
    k.dma(lb_in, lbl)
    lb = k.sb("lb", [128, 2], F32)
    oml = k.sb("oml", [128, 2], F32)
    k.tt(lb, lb_in[:, :, 0], lb_in[:, :, 1], ALU.subtract)
    k.act(lb, lb, AF.Sigmoid)
    k.ts(oml, lb, -1.0, ALU.mult, 1.0, ALU.add)
    NCK = TT // 64
    u_sb = [k.sb(f"u{i}", [128, DC, TT], BF16) for i in range(2)]
    ps_q = k.ps("ps_q"); ps_f = k.ps("ps_f"); ps_i = k.ps("ps_i"); ps_g = k.ps("ps_g")
    ps_o = k.ps("ps_o")
    ps_s = k.ps("ps_s", [64, NCK, 64], F32, units=NCK)
    ps_d = k.ps("ps_d", [128, 4, 128], F32, units=4)
    ps_t = k.ps("ps_t", [128, 4, 256], BF16, units=4)
    H = []
    for hh in range(2):
        h = dict(
            fval=k.sb(f"fval{hh}", [128, TT], F32), logf=k.sb(f"logf{hh}", [128, TT], F32),
            kval=k.sb(f"kval{hh}", [128, TT], F32), bcum=k.sb(f"bcum{hh}", [128, TT], F32),
            eb=k.sb(f"eb{hh}", [128, TT], F32), enb=k.sb(f"enb{hh}", [128, TT], F32),
            qtil=k.sb(f"qtil{hh}", [128, TT], BF16), ktil=k.sb(f"ktil{hh}", [128, TT], BF16),
            khT=k.sb(f"khT{hh}", [128, NCK, 64], BF16, units=NCK),
            iT=k.sb(f"iT{hh}", [128, TT], BF16), sg=k.sb(f"sg{hh}", [128, TT], BF16),
            tok=k.sb(f"tok{hh}", [64, 4, 256], BF16, units=4),
            sT=k.sb(f"sT{hh}", [64, 4, 64], BF16, units=4),
            st32=k.sb(f"st32{hh}", [128, 128], F32),
            stbf=k.sb(f"stbf{hh}", [128, 2, 128], BF16, units=2),
            o_sb=k.sb(f"o_sb{hh}", [128, TT], BF16),
        )
        H.append(h)
    cnt = [0, 0]
    ti = 0
    for b in range(B):
        for hh in range(2):
            k.memset(H[hh]["st32"], 0.0)
            k.memset(H[hh]["stbf"][cnt[hh] % 2], 0.0)
        for t in range(S // TT):
            t0 = b * S + t * TT
            u = u_sb[ti % 2]
            ti += 1
            for kc in range(DC):
                k.dma(u[:, kc, :], uT[kc * 128:(kc + 1) * 128, t0:t0 + TT], q=("sp" if kc % 2 == 0 else "act"))
            for hh in range(2):
                h = H[hh]
                for (ps, blk) in ((ps_f, 2 + hh), (ps_q, hh), (ps_i, 4 + hh), (ps_g, 6 + hh)):
                    for kc in range(DC):
                        k.mm(ps, w_sb[:, kc, blk * 128:(blk + 1) * 128], u[:, kc, :], start=(kc == 0), stop=(kc == DC - 1))
                k.act(h["fval"], ps_f, AF.Sigmoid)
                k.ts(h["fval"], h["fval"], oml[:, hh:hh + 1], ALU.mult, lb[:, hh:hh + 1], ALU.add)
                k.act(h["logf"], h["fval"], AF.Ln)
                k.ts(h["kval"], h["fval"], -1.0, ALU.mult, 1.0, ALU.add)
                bc, lf = h["bcum"], h["logf"]
                k.generic("dve", lambda e, bc=bc, lf=lf: e.tensor_tensor_scan(bc.ap, rmask.ap, lf.ap, 0.0, ALU.mult, ALU.add),
                          [rmask, lf], [bc])
                k.act(h["eb"], h["bcum"], AF.Exp)
                k.act(h["enb"], h["bcum"], AF.Exp, scale=-1.0)
                k.tt(h["qtil"], ps_q, h["eb"], ALU.mult)
                k.tt(h["ktil"], h["kval"], h["enb"], ALU.mult)
                k.copy(h["iT"], ps_i, eng="act")
                k.act(h["sg"], ps_g, AF.Silu)
                k.dma(sgT[hh * 128:(hh + 1) * 128, t0:t0 + TT], h["sg"], q="sp", is_output=True)
                for c in range(NCK):
                    cs = slice(c * 64, (c + 1) * 64)
                    last = h["eb"][:, c * 64 + 63:c * 64 + 64]
                    k.ts(h["khT"][c], h["ktil"][:, cs], last, ALU.mult)
                    pt = ps_t[c % 4]
                    k.transpose(pt[0:64, 0:128], h["khT"][c], cx.ident)
                    k.transpose(pt[0:64, 128:256], h["iT"][:, cs], cx.ident)
                    tok = h["tok"][c % 4]
                    k.copy(tok, pt[0:64, :], eng="dve")
                    k.mm(ps_s[c], h["ktil"][:, cs], h["qtil"][:, cs])
                    sT = h["sT"][c % 4]
                    k.tt(sT, ps_s[c], tri, ALU.mult)
                    sb_cur = h["stbf"][cnt[hh] % 2]
                    k.mm(ps_o[:, cs], sb_cur, h["qtil"][:, cs], start=True, stop=False)
                    k.mm(ps_o[:, cs], tok[:, 128:256], sT, start=False, stop=True)
                    pd = ps_d[c % 4]
                    k.mm(pd, tok[:, 0:128], tok[:, 128:256])
                    k.stt(h["st32"], h["st32"], last, pd, ALU.mult, ALU.add)
                    cnt[hh] += 1
                    k.copy(h["stbf"][cnt[hh] % 2], h["st32"], eng="act")
                k.copy(h["o_sb"], ps_o, eng="act")
                k.dma(oT[hh * 128:(hh + 1) * 128, t0:t0 + TT], h["o_sb"], q="sp", is_output=True)
    return k.finish()


MLA_SCALE = 192.0 ** -0.5
MASK_ENG = "dve"


def build_L4(B, S):
    k = K()
    T = B * S
    qn = k.dram("qn", [2, 128, T], BF16, "ExternalInput")
    qr = k.dram("qr", [2, 64, T], BF16, "ExternalInput")
    kn = k.dram("kn", [2, 128, T], BF16, "ExternalInput")
    kr = k.dram("kr", [64, T], BF16, "ExternalInput")
    vd = k.dram("v", [2, T, 128], BF16, "ExternalInput")
    maskd = k.dram("masks", [128, 4, 512], BF16, "ExternalInput")
    identd = k.dram("ident", [128, 128], BF16, "ExternalInput")
    oT = k.dram("oT", [2, T, 128], BF16, "ExternalOutput")
    cx = Ctx(k, identd)
    masks = k.sb("masks", [128, 4, 512], BF16)
    k.dma(masks, maskd)
    NKT = S // 128
    qn_sb = k.sb("qn_sb", [128, S], BF16)
    qr_sb = k.sb("qr_sb", [64, S], BF16)
    kn_sb = k.sb("kn_sb", [128, S], BF16)
    kr_sb = k.sb("kr_sb", [64, S], BF16)
    v_sb = k.sb("v_sb", [128, NKT, 136], BF16)
    k.memset(v_sb[:, :, 128:129], 1.0)
    ps_s = [k.ps(f"ps_s{i}") for i in range(2)]
    ps_o = [k.ps(f"ps_o{i}") for i in range(4)]
    ps_t = k.ps("ps_t", [128, 4, 128], BF16, units=4)
    pT = [k.sb(f"pT{i}", [128, 512], BF16) for i in range(3)]
    rden = k.sb("rden", [128, 4, 1], F32, units=4)
    on = k.sb("on", [128, 8, 128], BF16, units=8)
    o_sb = [k.sb(f"o_sb{i}", [128, 512], BF16) for i in range(2)]
    it = 0
    oi = 0
    for b in range(B):
        k.dma(kr_sb, kr[:, b * S:(b + 1) * S], q="sp")
        for hh in range(2):
            k.dma(qn_sb, qn[hh, :, b * S:(b + 1) * S], q="sp")
            k.dma(qr_sb, qr[hh, :, b * S:(b + 1) * S], q="act")
            k.dma(kn_sb, kn[hh, :, b * S:(b + 1) * S], q="sp")
            vv = vd[hh, b * S:(b + 1) * S, :].rearrange("(kt p) d -> p kt d", p=128)
            for k0 in range(0, NKT, 8):
                k.dma(v_sb[:, k0:k0 + 8, 0:128], vv[:, k0:k0 + 8, :], q=("act" if (k0 // 8) % 2 else "sp"))
            for qi in range(S // 512):
                qs = slice(qi * 512, (qi + 1) * 512)
                for kt in range(4 * qi + 4):
                    ks = slice(kt * 128, (kt + 1) * 128)
                    ps = ps_s[it % 2]
                    p = pT[it % 3]
                    it += 1
                    k.mm(ps, kn_sb[:, ks], qn_sb[:, qs], start=True, stop=False)
                    k.mm(ps, kr_sb[:, ks], qr_sb[:, qs], start=False, stop=True)
                    k.act(p, ps, AF.Exp, scale=MLA_SCALE)
                    j = kt - 4 * qi
                    if j >= 0:
                        k.tt(p, p, masks[:, j, :], ALU.mult, eng=MASK_ENG)
                    for sb in range(max(j, 0), 4):
                        k.mm(ps_o[sb][:, 0:129], p[:, sb * 128:(sb + 1) * 128], v_sb[:, kt, 0:129],
                             start=(kt == 0), stop=(kt == 4 * qi + sb))
                for sb in range(4):
                    k.recip(rden[sb], ps_o[sb][:, 128:129])
                    o_ = on[oi % 8]
                    oi += 1
                    k.ts(o_, ps_o[sb][:, 0:128], rden[sb], ALU.mult)
                    r0 = b * S + qi * 512 + sb * 128
                    k.dma(oT[hh, r0:r0 + 128, :], o_, q="sp", is_output=True)
    return k.finish()


class PsRot:
    def __init__(self, k, n, name="psr"):
        self.b = [k.ps(f"{name}{i}") for i in range(n)]
        self.i = 0

    def next(self):
        p = self.b[self.i % len(self.b)]
        self.i += 1
        return p


def linear(k, slabs, W, c0, nchunks, x_units, TT, psr, consume, width=128):
    KC = len(x_units)
    for j in range(nchunks):
        sl = slabs.load(W, c0 + j * width, width=width, kc=KC)
        ps = psr.next()
        for kc in range(KC):
            k.mm(ps[0:width, :TT], sl[:, kc, 0:width], x_units[kc][:, :TT], start=(kc == 0), stop=(kc == KC - 1))
        consume(j, ps)


def build_ffn(Tn, G=1024):
    k = K()
    uT = k.dram("uT", [D, Tn], BF16, "ExternalInput")
    wgu = k.dram("wgu", [D, 11264], F32, "ExternalInput")
    wdn = k.dram("wdn", [5632, D], F32, "ExternalInput")
    yT = k.dram("yT", [D, Tn], BF16, "ExternalOutput")
    FC = 44
    u_sb = k.sb("u_sb", [128, DC, G], BF16, units=DC)
    gated = k.sb("gated", [128, FC, G], BF16, units=FC)
    sl_gu = Slabs(k, "slgu", DC, 4)
    sl_d = Slabs(k, "sld", FC, 2)
    sgl = [k.sb(f"sgl{i}", [128, 512], BF16) for i in range(2)]
    ost = [k.sb(f"ost{i}", [128, 512], BF16) for i in range(3)]
    psr = PsRot(k, 8)
    n = 0
    for g in range(Tn // G):
        t0 = g * G
        for c in range(DC):
            k.dma(u_sb[c], uT[c * 128:(c + 1) * 128, t0:t0 + G], q=("sp" if c % 2 == 0 else "act"))
        for hb in range(FC):
            slg = sl_gu.load(wgu, hb * 128)
            slu = sl_gu.load(wgu, 5632 + hb * 128)
            for hf in range(G // 512):
                hs = slice(hf * 512, (hf + 1) * 512)
                pg = psr.next()
                pu = psr.next()
                for kc in range(DC):
                    k.mm(pg, slg[:, kc, :], u_sb[kc][:, hs], start=(kc == 0), stop=(kc == DC - 1))
                for kc in range(DC):
                    k.mm(pu, slu[:, kc, :], u_sb[kc][:, hs], start=(kc == 0), stop=(kc == DC - 1))
                s = sgl[n % 2]
                k.act(s, pg, AF.Silu)
                k.tt(gated[hb][:, hs], s, pu, ALU.mult)
                n += 1
        for dc in range(DC):
            sl = sl_d.load(wdn, dc * 128)
            for hf in range(G // 512):
                hs = slice(hf * 512, (hf + 1) * 512)
                ps = psr.next()
                for fc in range(FC):
                    k.mm(ps, sl[:, fc, :], gated[fc][:, hs], start=(fc == 0), stop=(fc == FC - 1))
                o = ost[n % 3]
                k.copy(o, ps, eng=("act" if n % 2 else "dve"))
                n += 1
                k.dma(yT[dc * 128:(dc + 1) * 128, t0 + hf * 512:t0 + (hf + 1) * 512], o, q="sp", is_output=True)
    return k.finish()


def build_post_mixer(Tc, hgrn, router, gate_c, shift_c, scale_c, TT=512):
    k = K()
    hT = k.dram("hT", [D, Tc], F32, "ExternalInput")
    oT = k.dram("oT", [D, Tc], BF16, "ExternalInput")
    W = k.dram("W", [D, D], F32, "ExternalInput")
    mods = k.dram("mods", [128, 224], F32, "ExternalInput")
    gains = k.dram("gains", [128, 3, DC], F32, "ExternalInput")
    houtT = k.dram("houtT", [D, Tc], F32, "ExternalOutput")
    uT = k.dram("uT", [D, Tc], BF16, "ExternalOutput")
    if hgrn:
        sgT = k.dram("sgT", [D, Tc], BF16, "ExternalInput")
    if router:
        wrd = k.dram("wr", [D, 8], F32, "ExternalInput")
        wd = k.dram("wd", [Tc, 8], F32, "ExternalOutput")
    cx = Ctx(k)
    m_sb = k.sb("m_sb", [128, 224], F32)
    g_sb = k.sb("g_sb", [128, 3, DC], F32)
    k.dma(m_sb, mods)
    k.dma(g_sb, gains)
    Gc = gate_coefs(k, "pm", m_sb, g_sb[:, 1, :], gate_c)
    A, B = mod_coefs(k, "pm", m_sb, g_sb[:, 2, :], shift_c, scale_c)
    o_sb = k.sb("o_sb", [128, DC, TT], BF16, units=DC)
    if hgrn:
        sg_sb = k.sb("sg_sb", [128, DC, TT], BF16, units=DC)
    sq = k.sb("sq", [128, DC, TT], BF16, units=DC)
    tmp = k.sb("tmp", [128, DC, TT], F32, units=DC)
    y_sb = k.sb("y_sb", [128, DC, TT], F32, units=DC)
    h_sb = k.sb("h_sb", [128, DC, TT], F32, units=DC)
    u_sb = k.sb("u_sb", [128, DC, TT], BF16, units=DC)
    rs = k.sb("rs", [128, TT], F32)
    slabs = Slabs(k, "slw", DC, 3)
    psr = PsRot(k, 6)
    ps_n = k.ps("ps_n")
    if router:
        wr_sb = k.sb("wr_sb", [128, DC, 8], F32)
        k.dma(wr_sb, wrd.rearrange("(kc p) e -> p kc e", p=128))
        ps_r = k.ps("ps_r", [128, 4, 8], F32, units=4)
        lg = k.sb("lg", [128, 4, 8], F32, units=4)
        top8 = k.sb("top8", [128, 4, 8], F32, units=4)
        msk = k.sb("msk", [128, 4, 8], F32, units=4)
        negm = k.sb("negm", [128, 4, 1], F32, units=4)
        ex = k.sb("ex", [128, 4, 8], F32, units=4)
        den = k.sb("den", [128, 4, 1], F32, units=4)
        wdt = k.sb("wdt", [128, 4, 8], F32, units=4)
    for t in range(Tc // TT):
        ts_ = slice(t * TT, (t + 1) * TT)
        for c in range(DC):
            k.dma(o_sb[c], oT[c * 128:(c + 1) * 128, ts_], q=("sp" if c % 2 == 0 else "act"))
        if hgrn:
            for c in range(DC):
                k.dma(sg_sb[c], sgT[c * 128:(c + 1) * 128, ts_], q=("sp" if c % 2 == 0 else "act"))
            cx.rstd(k, o_sb, TT, sq, ps_n, rs, D)
            for c in range(DC):
                k.tt(tmp[c], o_sb[c], rs, ALU.mult)
                k.stt(o_sb[c], tmp[c], g_sb[:, 0, c:c + 1], sg_sb[c], ALU.mult, ALU.mult)

        def cons(j, ps):
            k.copy(y_sb[j], ps, eng=("act" if j % 2 else "dve"))
        linear(k, slabs, W, 0, DC, o_sb, TT, psr, cons)
        cx.rstd(k, y_sb, TT, sq, ps_n, rs, D)
        for c in range(DC):
            k.dma(h_sb[c], hT[c * 128:(c + 1) * 128, ts_], q=("sp" if c % 2 == 0 else "act"))
        for c in range(DC):
            k.tt(tmp[c], y_sb[c], rs, ALU.mult)
            k.stt(h_sb[c], tmp[c], Gc[:, c:c + 1], h_sb[c], ALU.mult, ALU.add)
            k.dma(houtT[c * 128:(c + 1) * 128, ts_], h_sb[c], q="sp", is_output=True)
        cx.rstd(k, h_sb, TT, sq, ps_n, rs, D)
        norm_mod_apply(k, h_sb, rs, A, B, tmp, u_sb, TT, out2_units=(y_sb if router else None))
        for c in range(DC):
            k.dma(uT[c * 128:(c + 1) * 128, ts_], u_sb[c], q="sp", is_output=True)
        if router:
            for s4 in range(TT // 128):
                ss = slice(s4 * 128, (s4 + 1) * 128)
                for kc in range(DC):
                    k.mm(ps_r[s4], y_sb[kc][:, ss], wr_sb[:, kc, :], start=(kc == 0), stop=(kc == DC - 1))
                k.copy(lg[s4], ps_r[s4])
                a, b_ = top8[s4], lg[s4]
                k.generic("dve", lambda e, a=a, b_=b_: e.max(a.ap, b_.ap), [b_], [a])
                k.ts(msk[s4], lg[s4], top8[s4][:, 1:2], ALU.is_ge)
                k.ts(negm[s4], top8[s4][:, 0:1], -1.0, ALU.mult)
                k.act(ex[s4], lg[s4], AF.Exp, bias=negm[s4])
                k.tt(ex[s4], ex[s4], msk[s4], ALU.mult)
                d_, e_ = den[s4], ex[s4]
                k.generic("dve", lambda e, d_=d_, e_=e_: e.reduce_sum(d_.ap, e_.ap, mybir.AxisListType.X), [e_], [d_])
                k.recip(den[s4], den[s4])
                k.ts(wdt[s4], ex[s4], den[s4], ALU.mult)
                k.dma(wd[t * TT + s4 * 128:t * TT + (s4 + 1) * 128, :], wdt[s4], q="sp", is_output=True)
    return k.finish()

TWO_PI = 6.283185307179586
C1 = 6.28125
C2 = TWO_PI - C1


def build_post_ffn(Tc, moe, mla, gate_c, kv_cols=None, q_cols=None, TT=512):
    k = K()
    hT = k.dram("hT", [D, Tc], F32, "ExternalInput")
    mods = k.dram("mods", [128, 224], F32, "ExternalInput")
    gains = k.dram("gains", [128, 3, DC], F32, "ExternalInput")
    houtT = k.dram("houtT", [D, Tc], F32, "ExternalOutput")
    if moe:
        yaT = k.dram("yaT", [D, Tc], BF16, "ExternalInput")
        ybT = k.dram("ybT", [D, Tc], BF16, "ExternalInput")
        wab = k.dram("wab", [128, 2, Tc], F32, "ExternalInput")
    else:
        yT = k.dram("yT", [D, Tc], BF16, "ExternalInput")
    if mla:
        g4 = k.dram("g4", [128, 2, 4], F32, "ExternalInput")
        wkva = k.dram("wkva", [D, 576], F32, "ExternalInput")
        wkvb = k.dram("wkvb", [512, 4096], F32, "ExternalInput")
        wqa = k.dram("wqa", [D, 512], F32, "ExternalInput")
        wqb = k.dram("wqb", [512, 3072], F32, "ExternalInput")
        posd = k.dram("pos", [128, Tc], I32, "ExternalInput")
        invfd = k.dram("invf", [128, 1], F32, "ExternalInput")
        kvT = k.dram("kvT", [4096, Tc], BF16, "ExternalOutput")
        krT = k.dram("krT", [64, Tc], BF16, "ExternalOutput")
        qT = k.dram("qT", [3072, Tc], BF16, "ExternalOutput")
    cx = Ctx(k)
    m_sb = k.sb("m_sb", [128, 224], F32)
    g_sb = k.sb("g_sb", [128, 3, DC], F32)
    k.dma(m_sb, mods)
    k.dma(g_sb, gains)
    Gc = gate_coefs(k, "pf", m_sb, g_sb[:, 0, :], gate_c)
    h_sb = k.sb("h_sb", [128, DC, TT], F32, units=DC)
    sq = k.sb("sq", [128, DC, TT], BF16, units=DC)
    tmp = k.sb("tmp", [128, DC, TT], F32, units=DC)
    rs = k.sb("rs", [128, TT], F32)
    ps_n = k.ps("ps_n")
    if moe:
        ya_sb = k.sb("ya_sb", [128, DC, TT], BF16, units=DC)
        yb_sb = k.sb("yb_sb", [128, DC, TT], BF16, units=DC)
        y_sb = k.sb("y_sb", [128, DC, TT], F32, units=DC)
        w_sb = k.sb("w_sb", [128, 2, TT], F32)
    else:
        y_sb = k.sb("y_sb", [128, DC, TT], BF16, units=DC)
    if mla:
        g4_sb = k.sb("g4_sb", [128, 2, 4], F32)
        k.dma(g4_sb, g4)
        invf = k.sb("invf_sb", [128, 1], F32)
        k.dma(invf, invfd)
        Akv, Bkv = mod_coefs(k, "kv", m_sb, g_sb[:, 1, :], kv_cols[0], kv_cols[1])
        Aq, Bq = mod_coefs(k, "q", m_sb, g_sb[:, 2, :], q_cols[0], q_cols[1])
        un = k.sb("un", [128, DC, TT], BF16, units=DC)
        lat = k.sb("lat", [128, 4, TT], F32, units=4)
        latb = k.sb("latb", [128, 4, TT], BF16, units=4)
        kr12 = k.sb("kr12", [32, 2, TT], F32, units=2)
        qrp = k.sb("qrp", [128, 8, TT], F32, units=8)
        pos_i = k.sb("pos_i", [128, TT], I32)
        ang = k.sb("ang", [128, TT], F32)
        tq = k.sb("tq", [128, TT], F32)
        ki = k.sb("ki", [128, TT], I32)
        kf = k.sb("kf", [128, TT], F32)
        mk = k.sb("mk", [128, TT], F32)
        sin_t = k.sb("sin_t", [128, TT], F32)
        cos_t = k.sb("cos_t", [128, TT], F32)
        ra = k.sb("ra", [128, TT], F32)
        rb = k.sb("rb", [128, TT], F32)
        slabs = Slabs(k, "slw", DC, 3)
        slabs4 = Slabs(k, "slw4", 4, 3)
        psr = PsRot(k, 6)
        ost = [k.sb(f"ost{i}", [128, TT], BF16) for i in range(3)]
    n = 0
    for t in range(Tc // TT):
        ts_ = slice(t * TT, (t + 1) * TT)
        for c in range(DC):
            k.dma(h_sb[c], hT[c * 128:(c + 1) * 128, ts_], q=("sp" if c % 2 == 0 else "act"))
        if moe:
            k.dma(w_sb, wab[:, :, ts_])
            for c in range(DC):
                k.dma(ya_sb[c], yaT[c * 128:(c + 1) * 128, ts_], q="sp")
                k.dma(yb_sb[c], ybT[c * 128:(c + 1) * 128, ts_], q="act")
            for c in range(DC):
                k.tt(tmp[c], ya_sb[c], w_sb[:, 0, :], ALU.mult)
                k.tt(y_sb[c], yb_sb[c], w_sb[:, 1, :], ALU.mult)
                k.tt(y_sb[c], y_sb[c], tmp[c], ALU.add)
        else:
            for c in range(DC):
                k.dma(y_sb[c], yT[c * 128:(c + 1) * 128, ts_], q=("sp" if c % 2 == 1 else "act"))
        cx.rstd(k, y_sb, TT, sq, ps_n, rs, D)
        for c in range(DC):
            k.tt(tmp[c], y_sb[c], rs, ALU.mult)
            k.stt(h_sb[c], tmp[c], Gc[:, c:c + 1], h_sb[c], ALU.mult, ALU.add)
            k.dma(houtT[c * 128:(c + 1) * 128, ts_], h_sb[c], q="sp", is_output=True)
        if not mla:
            continue
        cx.rstd(k, h_sb, TT, sq, ps_n, rs, D)
        k.dma(pos_i, posd[:, ts_])
        k.copy(ang, pos_i)
        k.ts(ang, ang, invf[:, 0:1], ALU.mult)
        k.ts(tq, ang, 1.0 / TWO_PI, ALU.mult)
        k.copy(ki, tq)
        k.copy(kf, ki)
        k.stt(ang, kf, -C1, ang, ALU.mult, ALU.add)
        k.stt(ang, kf, -C2, ang, ALU.mult, ALU.add)
        k.ts(mk, ang, float(np.pi), ALU.is_gt)
        k.stt(ang, mk, -TWO_PI, ang, ALU.mult, ALU.add)
        k.ts(mk, ang, -float(np.pi), ALU.is_lt)
        k.stt(ang, mk, TWO_PI, ang, ALU.mult, ALU.add)
        k.act(sin_t, ang, AF.Sin)
        k.ts(tq, ang, float(np.pi / 2), ALU.add)
        k.ts(mk, tq, float(np.pi), ALU.is_gt)
        k.stt(tq, mk, -TWO_PI, tq, ALU.mult, ALU.add)
        k.act(cos_t, tq, AF.Sin)

        def rope(x1, x2, o1, o2, P):
            k.tt(ra[0:P], x1, cos_t[0:P], ALU.mult)
            k.tt(rb[0:P], x2, sin_t[0:P], ALU.mult)
            k.tt(o1, ra[0:P], rb[0:P], ALU.subtract)
            k.tt(ra[0:P], x2, cos_t[0:P], ALU.mult)
            k.tt(rb[0:P], x1, sin_t[0:P], ALU.mult)
            k.tt(o2, ra[0:P], rb[0:P], ALU.add)

        for c in range(DC):
            k.tt(tmp[c], h_sb[c], rs, ALU.mult)
            k.act(un[c], tmp[c], AF.Identity, bias=Bkv[:, c:c + 1], scale=Akv[:, c:c + 1])

        def cons_lat(j, ps):
            k.copy(lat[j], ps, eng=("act" if j % 2 else "dve"))
        linear(k, slabs, wkva, 0, 4, un, TT, psr, cons_lat)

        def cons_kr(j, ps):
            k.copy(kr12[j], ps[0:32, :], eng="act")
        linear(k, slabs, wkva, 512, 2, un, TT, psr, cons_kr, width=32)
        cx.rstd(k, lat, TT, sq, ps_n, rs2 := k_rs2(k), 512)
        for c in range(4):
            k.tt(lat[c], lat[c], rs2, ALU.mult)
            k.ts(latb[c], lat[c], g4_sb[:, 0, c:c + 1], ALU.mult)

        def cons_kv(j, ps):
            nonlocal n
            o = ost[n % 3]
            k.copy(o, ps, eng=("act" if n % 2 else "dve"))
            n += 1
            k.dma(kvT[j * 128:(j + 1) * 128, ts_], o, q="sp", is_output=True)
        linear(k, slabs4, wkvb, 0, 32, latb, TT, psr, cons_kv)
        o = ost[n % 3]
        n += 1
        rope(kr12[0], kr12[1], o[0:32], o[32:64], 32)
        k.dma(krT[:, ts_], o[0:64], q="sp", is_output=True)
        for c in range(DC):
            k.act(un[c], tmp[c], AF.Identity, bias=Bq[:, c:c + 1], scale=Aq[:, c:c + 1])
        linear(k, slabs, wqa, 0, 4, un, TT, psr, cons_lat)
        cx.rstd(k, lat, TT, sq, ps_n, rs2, 512)
        for c in range(4):
            k.tt(lat[c], lat[c], rs2, ALU.mult)
            k.ts(latb[c], lat[c], g4_sb[:, 1, c:c + 1], ALU.mult)

        def cons_q(j, ps):
            nonlocal n
            if j < 16:
                o = ost[n % 3]
                k.copy(o, ps, eng=("act" if n % 2 else "dve"))
                n += 1
                k.dma(qT[j * 128:(j + 1) * 128, ts_], o, q="sp", is_output=True)
            else:
                k.copy(qrp[j - 16], ps, eng=("act" if j % 2 else "dve"))
        linear(k, slabs4, wqb, 0, 24, latb, TT, psr, cons_q)
        for j in range(4):
            o1 = ost[n % 3]
            n += 1
            o2 = ost[n % 3]
            n += 1
            rope(qrp[j], qrp[4 + j], o1, o2, 128)
            k.dma(qT[2048 + j * 128:2048 + (j + 1) * 128, ts_], o1, q="sp", is_output=True)
            k.dma(qT[2560 + j * 128:2560 + (j + 1) * 128, ts_], o2, q="sp", is_output=True)
    return k.finish()


_rs2 = {}


def k_rs2(k):
    if id(k) not in _rs2:
        _rs2[id(k)] = k.sb("rs2", [128, 512], F32)
    return _rs2[id(k)]


import time as _time
import ml_dtypes
from concourse.bass_utils import run_bass_kernel_spmd

NCORES = 8
BF = ml_dtypes.bfloat16


def _fm(v):
    return np.ascontiguousarray(np.asarray(v, np.float32).reshape(-1, 128).T)


def _run(nc, in_maps, tag):
    t0 = _time.time()
    res = run_bass_kernel_spmd(nc, in_maps, core_ids=list(range(NCORES)))
    print(f"[kernel] {tag}: {_time.time() - t0:.1f}s", flush=True)
    return res.results


def kernel(x, c, positions, ada_w, ada_b, norm_g, hg_w_in, hg_lb_logits, hg_out_norm_g,
           hg_w_out, kv_src_norm_g, kv_src_ada_w, kv_src_ada_b, mla_w_kv_a, mla_kv_norm_g,
           mla_w_kv_b, mla_w_q_a, mla_q_norm_g, mla_w_q_b, mla_w_o, ffn_w_gu, ffn_w_down,
           moe_w_router, moe_w_gu, moe_w_down):
    f32 = np.float32
    x = np.asarray(x, f32)
    Bn, S, _ = x.shape
    T = Bn * S
    Tc = T // NCORES
    cpb = NCORES // Bn
    A = lambda a: np.asarray(a)
    ada_w, ada_b, norm_g = A(ada_w), A(ada_b), A(norm_g)
    ident = np.eye(128, dtype=f32).astype(BF)

    W_all = np.concatenate([ada_w[0, 0], ada_w[0, 1], ada_w[1, 0], ada_w[1, 1], A(kv_src_ada_w)], axis=1)
    b_all = np.concatenate([ada_b[0, 0], ada_b[0, 1], ada_b[1, 0], ada_b[1, 1], A(kv_src_ada_b)])
    NCH = W_all.shape[1] // 128 // NCORES
    cT = np.ascontiguousarray(A(c).astype(f32).T.reshape(DC, 128, Bn).transpose(1, 0, 2))
    nc = build_L0(NCH)
    ims = [{"cT": cT, "W": np.ascontiguousarray(W_all[:, i * NCH * 128:(i + 1) * NCH * 128]),
            "bias": _fm(b_all[i * NCH * 128:(i + 1) * NCH * 128])} for i in range(NCORES)]
    r = _run(nc, ims, "L0 mods")
    del W_all
    mods_all = np.concatenate([r[i]["out"] for i in range(NCORES)], axis=1)
    mods_b = [np.ascontiguousarray(mods_all[:, :, b]) for b in range(Bn)]

    def tok(i):
        b = i // cpb
        s0 = (i % cpb) * Tc
        return b, s0

    xT = []
    for i in range(NCORES):
        b, s0 = tok(i)
        xT.append(np.ascontiguousarray(x[b, s0:s0 + Tc, :].T))

    nc = build_L1(Tc)
    ims = [{"xT": xT[i], "mods": mods_b[tok(i)[0]], "gains": _fm(norm_g[0, 0, 0])} for i in range(NCORES)]
    r = _run(nc, ims, "L1 norm0")
    u0T = np.concatenate([r[i]["uT"] for i in range(NCORES)], axis=1)

    w_in = A(hg_w_in)[0]
    lbl_all = A(hg_lb_logits).astype(f32)
    tri = np.triu(np.ones((64, 64), f32))
    rmask = np.ones((128, 512), f32)
    rmask[:, ::64] = 0
    nc = build_L2(Bn, S)
    ims = []
    for i in range(NCORES):
        hs = (2 * i, 2 * i + 1)
        blocks = []
        for base in (0, 2048, 4096, 6144):
            for h in hs:
                blocks.append(w_in[:, base + h * 128: base + (h + 1) * 128])
        lbl = np.stack([np.stack([lbl_all[0, h * 128:(h + 1) * 128], lbl_all[1, h * 128:(h + 1) * 128]], -1) for h in hs], 1)
        ims.append({"uT": u0T, "Wc": np.ascontiguousarray(np.concatenate(blocks, 1)), "lbl": np.ascontiguousarray(lbl.astype(f32)),
                    "ident": ident, "tri": tri, "rmask": rmask})
    r = _run(nc, ims, "L2 hgrn2")
    del u0T
    oT_all = np.concatenate([r[i]["oT"] for i in range(NCORES)], axis=0)
    sgT_all = np.concatenate([r[i]["sgT"] for i in range(NCORES)], axis=0)

    def cols(a, i):
        return np.ascontiguousarray(a[:, i * Tc:(i + 1) * Tc])

    nc = build_post_mixer(Tc, True, False, 32, 48, 64)
    g3 = np.ascontiguousarray(np.stack([_fm(A(hg_out_norm_g)[0]), _fm(norm_g[0, 0, 1]), _fm(norm_g[0, 1, 0])], 1))
    w_out = np.ascontiguousarray(A(hg_w_out)[0])
    ims = [{"hT": xT[i], "oT": cols(oT_all, i), "sgT": cols(sgT_all, i), "W": w_out, "mods": mods_b[tok(i)[0]], "gains": g3}
           for i in range(NCORES)]
    r = _run(nc, ims, "L3a post-hgrn2")
    del oT_all, sgT_all, xT
    h1T = [r[i]["houtT"] for i in range(NCORES)]
    u1T = [r[i]["uT"] for i in range(NCORES)]

    nc = build_ffn(Tc)
    wgu = np.ascontiguousarray(A(ffn_w_gu)[0])
    wdn = np.ascontiguousarray(A(ffn_w_down)[0])
    ims = [{"uT": u1T[i], "wgu": wgu, "wdn": wdn} for i in range(NCORES)]
    r = _run(nc, ims, "FFN dense")
    y2T = [r[i]["yT"] for i in range(NCORES)]
    del u1T, wgu, wdn

    nc = build_post_ffn(Tc, False, True, 80, kv_cols=(192, 208), q_cols=(96, 112))
    g3 = np.ascontiguousarray(np.stack([_fm(norm_g[0, 1, 1]), _fm(A(kv_src_norm_g)), _fm(norm_g[1, 0, 0])], 1))
    g4 = np.ascontiguousarray(np.stack([_fm(A(mla_kv_norm_g)), _fm(A(mla_q_norm_g)[0])], 1))
    perm = np.concatenate([[h * 192 + d for h in range(16) for d in range(128)],
                           [h * 192 + 128 + j for h in range(16) for j in range(32)],
                           [h * 192 + 160 + j for h in range(16) for j in range(32)]]).astype(np.int64)
    wqb = np.ascontiguousarray(A(mla_w_q_b)[0][:, perm])
    invf32 = (1.0 / (10000.0 ** (np.arange(0, 64, 2, dtype=f32) / 64))).astype(f32)
    invf = np.ascontiguousarray(np.concatenate([invf32] * 4).reshape(128, 1))
    pos = A(positions).astype(np.int32)
    ims = []
    for i in range(NCORES):
        b, s0 = tok(i)
        ims.append({"hT": h1T[i], "mods": mods_b[b], "gains": g3, "yT": y2T[i], "g4": g4,
                    "wkva": np.ascontiguousarray(A(mla_w_kv_a)), "wkvb": np.ascontiguousarray(A(mla_w_kv_b)),
                    "wqa": np.ascontiguousarray(A(mla_w_q_a)[0]), "wqb": wqb,
                    "pos": np.ascontiguousarray(np.broadcast_to(pos[b, s0:s0 + Tc][None], (128, Tc))), "invf": invf})
    r = _run(nc, ims, "L3c post-ffn + mla proj")
    del h1T, y2T
    h2T = [r[i]["houtT"] for i in range(NCORES)]
    kvT = np.concatenate([r[i]["kvT"] for i in range(NCORES)], axis=1)
    krT = np.ascontiguousarray(np.concatenate([r[i]["krT"] for i in range(NCORES)], axis=1))
    qT = np.concatenate([r[i]["qT"] for i in range(NCORES)], axis=1)

    kk = np.arange(128)[:, None, None]
    jj = np.arange(4)[None, :, None]
    qq = np.arange(512)[None, None, :]
    masks = ((kk + 128 * jj) <= qq).astype(f32).astype(BF)
    nc = build_L4(Bn, S)
    ims = []
    for i in range(NCORES):
        hs = (2 * i, 2 * i + 1)
        ims.append({
            "qn": np.ascontiguousarray(np.stack([qT[h * 128:(h + 1) * 128] for h in hs])),
            "qr": np.ascontiguousarray(np.stack([np.concatenate([qT[2048 + h * 32:2048 + (h + 1) * 32], qT[2560 + h * 32:2560 + (h + 1) * 32]], 0) for h in hs])),
            "kn": np.ascontiguousarray(np.stack([kvT[h * 256:h * 256 + 128] for h in hs])),
            "kr": krT,
            "v": np.ascontiguousarray(np.stack([kvT[h * 256 + 128:h * 256 + 256].T for h in hs])),
            "masks": masks, "ident": ident})
    r = _run(nc, ims, "L4 attention")
    del kvT, qT
    aT_all = np.concatenate([r[i]["oT"][hh].T for i in range(NCORES) for hh in range(2)], axis=0)

    nc = build_post_mixer(Tc, False, True, 128, 144, 160)
    g3 = np.ascontiguousarray(np.stack([_fm(np.ones(D, f32)), _fm(norm_g[1, 0, 1]), _fm(norm_g[1, 1, 0])], 1))
    w_o = np.ascontiguousarray(A(mla_w_o)[0])
    wr = np.ascontiguousarray(A(moe_w_router)[0].astype(f32))
    ims = [{"hT": h2T[i], "oT": cols(aT_all, i), "W": w_o, "mods": mods_b[tok(i)[0]], "gains": g3, "wr": wr} for i in range(NCORES)]
    r = _run(nc, ims, "L5 post-attn + router")
    del aT_all, h2T
    h3T = [r[i]["houtT"] for i in range(NCORES)]
    u3T = np.concatenate([r[i]["uT"] for i in range(NCORES)], axis=1)
    wd = np.concatenate([r[i]["wd"] for i in range(NCORES)], axis=0)

    sel = wd > 0
    NE = sel.shape[1]
    rank = np.cumsum(sel, axis=1)
    first = sel & (rank == 1)
    second = sel & (rank == 2)
    toks = [np.nonzero(first[:, e] | second[:, e])[0] for e in range(NE)]
    nmax = max(1, max(len(tk) for tk in toks))
    CAP = ((nmax + 1023) // 1024) * 1024
    print(f"[kernel] expert loads {[len(tk) for tk in toks]} CAP={CAP}", flush=True)
    nc = build_ffn(CAP)
    mgu, mdn = A(moe_w_gu)[0], A(moe_w_down)[0]
    ims = []
    for e in range(NE):
        ug = np.zeros((D, CAP), BF)
        ug[:, :len(toks[e])] = u3T[:, toks[e]]
        ims.append({"uT": ug, "wgu": np.ascontiguousarray(mgu[e]), "wdn": np.ascontiguousarray(mdn[e])})
    r = _run(nc, ims, "FFN experts")
    del u3T
    yaT = np.zeros((D, T), BF)
    ybT = np.zeros((D, T), BF)
    wa = np.zeros(T, f32)
    wb = np.zeros(T, f32)
    for e in range(NE):
        tk = toks[e]
        ye = r[e]["yT"][:, :len(tk)]
        f1 = first[tk, e]
        yaT[:, tk[f1]] = ye[:, f1]
        ybT[:, tk[~f1]] = ye[:, ~f1]
        wa[tk[f1]] = wd[tk[f1], e]
        wb[tk[~f1]] = wd[tk[~f1], e]

    nc = build_post_ffn(Tc, True, False, 176)
    g3 = np.ascontiguousarray(np.stack([_fm(norm_g[1, 1, 1]), _fm(np.ones(D, f32)), _fm(np.ones(D, f32))], 1))
    ims = []
    for i in range(NCORES):
        sl = slice(i * Tc, (i + 1) * Tc)
        wab = np.ascontiguousarray(np.broadcast_to(np.stack([wa[sl], wb[sl]])[None], (128, 2, Tc)))
        ims.append({"hT": h3T[i], "mods": mods_b[tok(i)[0]], "gains": g3, "yaT": cols(yaT, i), "ybT": cols(ybT, i), "wab": wab})
    r = _run(nc, ims, "L7 moe combine")
    out = np.empty((Bn, S, D), f32)
    for i in range(NCORES):
        b, s0 = tok(i)
        out[b, s0:s0 + Tc, :] = r[i]["houtT"].T
    return out
```

```python
import numpy as np
from contextlib import ExitStack
import concourse.bass as bass
import concourse.mybir as mybir

F32 = mybir.dt.float32
BF16 = mybir.dt.bfloat16
I32 = mybir.dt.int32
AF = mybir.ActivationFunctionType
ALU = mybir.AluOpType

ENGS = ("pe", "act", "dve", "pool", "sp")
import os
NDMASEM = 12


class Unit:
    __slots__ = ("name", "writer", "readers", "dreaders")

    def __init__(self, name):
        self.name = name
        self.writer = None
        self.readers = {}
        self.dreaders = []


class V:
    __slots__ = ("u", "ap")

    def __init__(self, u, ap):
        self.u = u
        self.ap = ap

    def __getitem__(self, idx):
        return V(self.u, self.ap[idx])


class Op:
    __slots__ = ("eng", "fn", "waits", "is_dma", "signal", "idx", "dsem", "dval", "cnt", "qi")

    def __init__(self, eng, fn, is_dma):
        self.eng = eng
        self.fn = fn
        self.is_dma = is_dma
        self.waits = []
        self.signal = False
        self.cnt = None


class K:
    def __init__(self, name="k"):
        self.nc = bass.Bass("TRN2", target_bir_lowering=False)
        self.es = ExitStack()
        self.ops = {e: [] for e in ENGS}
        self.ndma = {e: 0 for e in ENGS}
        self.dma_ops = {e: [] for e in ENGS}
        self.n = 0
        self.out_dmas = []

    def dram(self, name, shape, dt, kind):
        return self.nc.dram_tensor(name, list(shape), dt, kind=kind).ap()

    def sb(self, name, shape, dt=F32, units=1):
        t = self.es.enter_context(self.nc.sbuf_tensor("s_" + name, list(shape), dt))
        if units == 1:
            return V(Unit(name), t[:])
        return [V(Unit(f"{name}{j}"), t[:, j]) for j in range(units)]

    def ps(self, name, shape=(128, 512), dt=F32, units=1):
        t = self.es.enter_context(self.nc.psum_tensor("p_" + name, list(shape), dt))
        if units == 1:
            return V(Unit(name), t[:])
        return [V(Unit(f"{name}{j}"), t[:, j]) for j in range(units)]

    def _rec(self, eng, fn, reads, writes, is_dma=False):
        op = Op(eng, fn, is_dma)
        op.idx = len(self.ops[eng])
        deps = []
        for v in reads:
            u = v.u
            if u.writer is not None:
                deps.append((u.writer, "raw"))
        for v in writes:
            u = v.u
            if u.writer is not None:
                deps.append((u.writer, "waw"))
            for r in list(u.readers.values()) + u.dreaders:
                deps.append((r, "war"))
        seen = set()
        for p, kind in deps:
            if p is op or id(p) in seen:
                continue
            if (not p.is_dma) and p.eng == eng and not is_dma:
                if eng == "pe" or kind == "war":
                    continue
            seen.add(id(p))
            op.waits.append(p)
            p.signal = True
        for v in reads:
            if is_dma:
                v.u.dreaders.append(op)
            else:
                v.u.readers[eng] = op
        for v in writes:
            v.u.writer = op
            v.u.readers = {}
            v.u.dreaders = []
        if is_dma:
            op.qi = self.ndma[eng]
            self.ndma[eng] += 1
            self.dma_ops[eng].append(op)
            op.signal = True
        self.ops[eng].append(op)
        return op

    def eng_obj(self, e):
        nc = self.nc
        return {"pe": nc.tensor, "act": nc.scalar, "dve": nc.vector, "pool": nc.gpsimd, "sp": nc.sync}[e]

    def mm(self, out, lhsT, rhs, start=True, stop=True):
        return self._rec("pe", lambda e: e.matmul(out.ap, lhsT.ap, rhs.ap, start=start, stop=stop),
                         [lhsT, rhs] + ([] if start else [out]), [out])

    def transpose(self, out, in_, ident):
        return self._rec("pe", lambda e: e.transpose(out.ap, in_.ap, ident.ap), [in_, ident], [out])

    def act(self, out, in_, func, bias=None, scale=None, extra_reads=()):
        kw = {}
        rd = [in_] + list(extra_reads)
        if bias is not None:
            if isinstance(bias, V):
                kw["bias"] = bias.ap
                rd.append(bias)
            else:
                kw["bias"] = bias
        if scale is not None:
            if isinstance(scale, V):
                kw["scale"] = scale.ap
                rd.append(scale)
            else:
                kw["scale"] = scale
        return self._rec("act", lambda e: e.activation(out.ap, in_.ap, func, **kw), rd, [out])

    def tt(self, out, in0, in1, op, eng="dve"):
        return self._rec(eng, lambda e: e.tensor_tensor(out.ap, in0.ap, in1.ap, op), [in0, in1], [out])

    def ts(self, out, in0, s1, op0, s2=None, op1=None, eng="dve"):
        rd = [in0]
        a1 = s1
        a2 = s2
        if isinstance(s1, V):
            rd.append(s1)
            a1 = s1.ap
        if isinstance(s2, V):
            rd.append(s2)
            a2 = s2.ap
        if op1 is None:
            return self._rec(eng, lambda e: e.tensor_scalar(out.ap, in0.ap, a1, None, op0), rd, [out])
        return self._rec(eng, lambda e: e.tensor_scalar(out.ap, in0.ap, a1, a2, op0, op1), rd, [out])

    def stt(self, out, in0, scalar, in1, op0, op1):
        rd = [in0, in1]
        a = scalar
        if isinstance(scalar, V):
            rd.append(scalar)
            a = scalar.ap
        return self._rec("dve", lambda e: e.scalar_tensor_tensor(out.ap, in0.ap, a, in1.ap, op0, op1), rd, [out])

    def copy(self, out, in_, eng="dve"):
        if eng == "act":
            return self._rec("act", lambda e: e.copy(out.ap, in_.ap), [in_], [out])
        return self._rec(eng, lambda e: e.tensor_copy(out.ap, in_.ap), [in_], [out])

    def memset(self, out, val, eng="dve"):
        return self._rec(eng, lambda e: e.memset(out.ap, val), [], [out])

    def recip(self, out, in_):
        return self._rec("dve", lambda e: e.reciprocal(out.ap, in_.ap), [in_], [out])

    def generic(self, eng, fn, reads, writes):
        return self._rec(eng, fn, reads, writes)

    def dma(self, out, in_, q="sp", is_output=False):
        rd = [in_] if isinstance(in_, V) else []
        wr = [out] if isinstance(out, V) else []
        oa = out.ap if isinstance(out, V) else out
        ia = in_.ap if isinstance(in_, V) else in_
        op = self._rec(q, lambda e: e.dma_start(out=oa, in_=ia), rd, wr, is_dma=True)
        if is_output:
            self.out_dmas.append(op)
        return op

    def finish(self):
        nc = self.nc
        es = self.es
        csem = {e: es.enter_context(nc.semaphore(f"c_{e}")) for e in ENGS}
        dsem = {e: [es.enter_context(nc.semaphore(f"d_{e}{i}")) for i in range(NDMASEM)]
                for e in ENGS if self.ndma[e] > 0}
        for e in ENGS:
            c = 0
            for op in self.ops[e]:
                if op.is_dma:
                    op.dsem = dsem[e][op.qi % NDMASEM]
                    op.dval = 16 * (op.qi // NDMASEM + 1)
                elif op.signal:
                    c += 1
                    op.cnt = c
        final_waits = list(self.out_dmas)
        block = es.enter_context(nc.Block())

        def emit(e):
            eng = self.eng_obj(e)
            seen_c = {x: 0 for x in ENGS}
            seen_d = {}
            oplist = self.ops[e]

            def wait_on(p):
                if p.is_dma:
                    key = id(p.dsem)
                    if seen_d.get(key, 0) >= p.dval:
                        return
                    eng.wait_ge(p.dsem, p.dval)
                    seen_d[key] = p.dval
                else:
                    if seen_c[p.eng] >= p.cnt:
                        return
                    eng.wait_ge(csem[p.eng], p.cnt)
                    seen_c[p.eng] = p.cnt

            for op in oplist:
                for p in op.waits:
                    wait_on(p)
                if op.is_dma and op.qi >= NDMASEM:
                    wait_on(self.dma_ops[e][op.qi - NDMASEM])
                ins = op.fn(eng)
                if op.is_dma:
                    ins.then_inc(op.dsem, 16)
                elif op.signal:
                    ins.then_inc(csem[e], 1)
            if e == "sp":
                for p in final_waits:
                    wait_on(p)

        block.sync(lambda s: emit("sp"))
        block.tensor(lambda s: emit("pe"))
        block.scalar(lambda s: emit("act"))
        block.vector(lambda s: emit("dve"))
        block.gpsimd(lambda s: emit("pool"))
        es.close()
        return nc


D = 2048
DC = 16
EPS = 1e-6


def chunked(ap, p=128):
    return ap.rearrange("(c p) t -> p c t", p=p)


class Ctx:
    def __init__(self, k, ident_dram=None):
        self.k = k
        self.ones = k.sb("ones_bf", [128, 128], BF16)
        k.memset(self.ones, 1.0)
        self.eps = k.sb("eps_t", [128, 1], F32)
        k.memset(self.eps, EPS)
        if ident_dram is not None:
            self.ident = k.sb("ident_bf", [128, 128], BF16)
            k.dma(self.ident, ident_dram, q="pool")
        self.pi = 0

    def rstd(self, k, x_units, TT, sq_units, ps, out, nfeat):
        n = len(x_units)
        for c in range(n):
            k.act(sq_units[c][:, :TT], x_units[c][:, :TT], AF.Square)
        for c in range(n):
            k.mm(ps[:, :TT], self.ones, sq_units[c][:, :TT], start=(c == 0), stop=(c == n - 1))
        k.act(out[:, :TT], ps[:, :TT], AF.Sqrt, bias=self.eps, scale=1.0 / nfeat)
        k.recip(out[:, :TT], out[:, :TT])


def mod_coefs(k, name, mods, g, shift_c, scale_c, n=DC):
    A = k.sb(name + "_A", [128, n], F32)
    k.stt(A, mods[:, scale_c:scale_c + n], 1.0, g, ALU.add, ALU.mult)
    return A, mods[:, shift_c:shift_c + n]


def gate_coefs(k, name, mods, g, gate_c, n=DC):
    G = k.sb(name + "_G", [128, n], F32)
    k.tt(G, mods[:, gate_c:gate_c + n], g, ALU.mult)
    return G


def norm_mod_apply(k, x_units, rstd, A, B, tmp_units, out_units, TT, out2_units=None):
    n = len(x_units)
    for c in range(n):
        k.tt(tmp_units[c][:, :TT], x_units[c][:, :TT], rstd[:, :TT], ALU.mult)
        k.act(out_units[c][:, :TT], tmp_units[c][:, :TT], AF.Identity, bias=B[:, c:c + 1], scale=A[:, c:c + 1])
        if out2_units is not None:
            k.ts(out2_units[c][:, :TT], tmp_units[c][:, :TT], A[:, c:c + 1], ALU.mult, B[:, c:c + 1], ALU.add)


class Slabs:
    def __init__(self, k, name, KC, nbuf, width=128):
        self.k = k
        self.bufs = [k.sb(f"{name}{i}", [128, KC, width], BF16) for i in range(nbuf)]
        self.i = 0
        self.KC = KC

    def load(self, W, c0, width=128, kc=None):
        b = self.bufs[self.i % len(self.bufs)]
        self.i += 1
        kc = kc or self.KC
        self.k.dma(b[:, :kc, :width], W.rearrange("(kc p) n -> p kc n", p=128)[:, :, c0:c0 + width], q="pool")
        return b


def build_L0(NCH=28):
    k = K()
    cT = k.dram("cT", [128, DC, 2], F32, "ExternalInput")
    W = k.dram("W", [D, NCH * 128], F32, "ExternalInput")
    bias = k.dram("bias", [128, NCH], F32, "ExternalInput")
    out = k.dram("out", [128, NCH, 2], F32, "ExternalOutput")
    c_sb = k.sb("c_sb", [128, DC, 2], F32)
    sc = k.sb("sc", [128, DC, 2], F32)
    b_sb = k.sb("b_sb", [128, NCH], F32)
    o_sb = k.sb("o_sb", [128, NCH, 2], F32)
    slabs = [k.sb(f"sl{i}", [128, DC, 128], F32) for i in range(4)]
    pss = [k.ps(f"ps{i}") for i in range(4)]
    k.dma(c_sb, cT)
    k.dma(b_sb, bias)
    k.act(sc, c_sb, AF.Silu)
    Wv = W.rearrange("(kc p) n -> p kc n", p=128)
    for j in range(NCH):
        sl = slabs[j % 4]
        k.dma(sl, Wv[:, :, j * 128:(j + 1) * 128], q=("sp" if j % 2 == 0 else "act"))
        ps = pss[j % 4]
        for kc in range(DC):
            k.mm(ps[:, 0:2], sl[:, kc, :], sc[:, kc, :], start=(kc == 0), stop=(kc == DC - 1))
        k.ts(o_sb[:, j, :], ps[:, 0:2], b_sb[:, j:j + 1], ALU.add)
    k.dma(out, o_sb, is_output=True)
    return k.finish()


def build_L1(Tc, TT=512):
    k = K()
    xT = k.dram("xT", [D, Tc], F32, "ExternalInput")
    mods = k.dram("mods", [128, 224], F32, "ExternalInput")
    gains = k.dram("gains", [128, DC], F32, "ExternalInput")
    uT = k.dram("uT", [D, Tc], BF16, "ExternalOutput")
    cx = Ctx(k)
    m_sb = k.sb("m_sb", [128, 224], F32)
    g_sb = k.sb("g_sb", [128, DC], F32)
    k.dma(m_sb, mods)
    k.dma(g_sb, gains)
    A, B = mod_coefs(k, "n0", m_sb, g_sb, 0, 16)
    NB = 2
    xs = [k.sb(f"x{b}", [128, DC, TT], F32, units=DC) for b in range(NB)]
    sq = k.sb("sq", [128, DC, TT], BF16, units=DC)
    tmp = k.sb("tmp", [128, DC, TT], F32, units=DC)
    us = [k.sb(f"u{b}", [128, DC, TT], BF16, units=DC) for b in range(NB)]
    rs = [k.sb(f"rs{b}", [128, TT], F32) for b in range(NB)]
    pss = [k.ps(f"ps{i}") for i in range(2)]
    xv = chunked(xT)
    uv = chunked(uT)
    for t in range(Tc // TT):
        b = t % NB
        for c in range(DC):
            k.dma(xs[b][c], xv[:, c, t * TT:(t + 1) * TT], q=("sp" if c % 2 == 0 else "act"))
        cx.rstd(k, xs[b], TT, sq, pss[b], rs[b], D)
        norm_mod_apply(k, xs[b], rs[b], A, B, tmp, us[b], TT)
        for c in range(DC):
            k.dma(uv[:, c, t * TT:(t + 1) * TT], us[b][c], q="sp", is_output=True)
    return k.finish()


def build_L2(B, S, TT=512):
    k = K()
    T = B * S
    uT = k.dram("uT", [D, T], BF16, "ExternalInput")
    Wc = k.dram("Wc", [D, 1024], F32, "ExternalInput")
    lbl = k.dram("lbl", [128, 2, 2], F32, "ExternalInput")
    identd = k.dram("ident", [128, 128], BF16, "ExternalInput")
    trid = k.dram("tri", [64, 64], F32, "ExternalInput")
    rmaskd = k.dram("rmask", [128, TT], F32, "ExternalInput")
    oT = k.dram("oT", [256, T], BF16, "ExternalOutput")
    sgT = k.dram("sgT", [256, T], BF16, "ExternalOutput")
    cx = Ctx(k, identd)
    tri = k.sb("tri", [64, 64], F32)
    k.dma(tri, trid)
    rmask = k.sb("rmask", [128, TT], F32)
    k.dma(rmask, rmaskd)
    w_sb = k.sb("w_sb", [128, DC, 1024], BF16)
    for kc in range(DC):
        k.dma(w_sb[:, kc, :], Wc[kc * 128:(kc + 1) * 128, :], q="pool")
    lb_in = k.sb("lb_in", [128, 2, 2], F32)
    k.dma(lb_in, lbl)
    lb = k.sb("lb", [128, 2], F32)
    oml = k.sb("oml", [128, 2], F32)
    k.tt(lb, lb_in[:, :, 0], lb_in[:, :, 1], ALU.subtract)
    k.act(lb, lb, AF.Sigmoid)
    k.ts(oml, lb, -1.0, ALU.mult, 1.0, ALU.add)
    NCK = TT // 64
    u_sb = [k.sb(f"u{i}", [128, DC, TT], BF16) for i in range(2)]
    ps_p = [k.ps(f"ps_p{i}") for i in range(3)]
    ps_o = [k.ps(f"ps_o{i}") for i in range(2)]
    ps_s = k.ps("ps_s", [64, 8, 64], F32, units=8)
    ps_d = k.ps("ps_d", [128, 4, 128], F32, units=4)
    ps_t = k.ps("ps_t", [128, 4, 256], BF16, units=4)
    H = []
    for hh in range(2):
        h = dict(
            fval=k.sb(f"fval{hh}", [128, TT], F32), logf=k.sb(f"logf{hh}", [128, TT], F32),
            kval=k.sb(f"kval{hh}", [128, TT], F32), bcum=k.sb(f"bcum{hh}", [128, TT], F32),
            eb=k.sb(f"eb{hh}", [128, TT], F32), enb=k.sb(f"enb{hh}", [128, TT], F32),
            q32=k.sb(f"q32{hh}", [128, TT], F32),
            qtil=k.sb(f"qtil{hh}", [128, TT], BF16), ktil=k.sb(f"ktil{hh}", [128, TT], BF16),
            khT=k.sb(f"khT{hh}", [128, NCK, 64], BF16, units=NCK),
            iT=k.sb(f"iT{hh}", [128, TT], BF16), sg=k.sb(f"sg{hh}", [128, TT], BF16),
            tok=k.sb(f"tok{hh}", [64, 4, 256], BF16, units=4),
            sT=k.sb(f"sT{hh}", [64, 4, 64], BF16, units=4),
            st32=k.sb(f"st32{hh}", [128, 128], F32),
            stbf=k.sb(f"stbf{hh}", [128, 2, 128], BF16, units=2),
            o_sb=k.sb(f"o_sb{hh}", [128, TT], BF16),
        )
        H.append(h)
    cnt = [0, 0]
    ti = 0
    pn = 0
    for b in range(B):
        for hh in range(2):
            k.memset(H[hh]["st32"], 0.0)
            k.memset(H[hh]["stbf"][cnt[hh] % 2], 0.0)
        for t in range(S // TT):
            t0 = b * S + t * TT
            u = u_sb[ti % 2]
            ti += 1
            for kc in range(DC):
                k.dma(u[:, kc, :], uT[kc * 128:(kc + 1) * 128, t0:t0 + TT], q=("sp" if kc % 2 == 0 else "act"))
            for hh in range(2):
                h = H[hh]
                for (kind, blk) in (("f", 2 + hh), ("q", hh), ("i", 4 + hh), ("g", 6 + hh)):
                    ps = ps_p[pn % 3]
                    pn += 1
                    for kc in range(DC):
                        k.mm(ps, w_sb[:, kc, blk * 128:(blk + 1) * 128], u[:, kc, :], start=(kc == 0), stop=(kc == DC - 1))
                    if kind == "f":
                        k.act(h["fval"], ps, AF.Sigmoid)
                    elif kind == "q":
                        k.copy(h["q32"], ps, eng="dve")
                    elif kind == "i":
                        k.copy(h["iT"], ps, eng="act")
                    else:
                        k.act(h["sg"], ps, AF.Silu)
                        k.dma(sgT[hh * 128:(hh + 1) * 128, t0:t0 + TT], h["sg"], q="sp", is_output=True)
            for hh in range(2):
                h = H[hh]
                k.ts(h["fval"], h["fval"], oml[:, hh:hh + 1], ALU.mult, lb[:, hh:hh + 1], ALU.add)
                k.act(h["logf"], h["fval"], AF.Ln)
                k.ts(h["kval"], h["fval"], -1.0, ALU.mult, 1.0, ALU.add)
                bc, lf = h["bcum"], h["logf"]
                k.generic("dve", lambda e, bc=bc, lf=lf: e.tensor_tensor_scan(bc.ap, rmask.ap, lf.ap, 0.0, ALU.mult, ALU.add),
                          [rmask, lf], [bc])
                k.act(h["eb"], h["bcum"], AF.Exp)
                k.act(h["enb"], h["bcum"], AF.Exp, scale=-1.0)
                k.tt(h["qtil"], h["q32"], h["eb"], ALU.mult)
                k.tt(h["ktil"], h["kval"], h["enb"], ALU.mult)
            for c in range(NCK):
                cs = slice(c * 64, (c + 1) * 64)
                for hh in range(2):
                    h = H[hh]
                    ui = 2 * c + hh
                    last = h["eb"][:, c * 64 + 63:c * 64 + 64]
                    k.ts(h["khT"][c], h["ktil"][:, cs], last, ALU.mult)
                    pt = ps_t[ui % 4]
                    k.transpose(pt[0:64, 0:128], h["khT"][c], cx.ident)
                    k.transpose(pt[0:64, 128:256], h["iT"][:, cs], cx.ident)
                    tok = h["tok"][c % 4]
                    k.copy(tok, pt[0:64, :], eng="act")
                    pss = ps_s[ui % 8]
                    k.mm(pss, h["ktil"][:, cs], h["qtil"][:, cs])
                    sT = h["sT"][c % 4]
                    k.tt(sT, pss, tri, ALU.mult)
                    sb_cur = h["stbf"][cnt[hh] % 2]
                    po = ps_o[hh]
                    k.mm(po[:, cs], sb_cur, h["qtil"][:, cs], start=True, stop=False)
                    k.mm(po[:, cs], tok[:, 128:256], sT, start=False, stop=True)
                    pd = ps_d[ui % 4]
                    k.mm(pd, tok[:, 0:128], tok[:, 128:256])
                    k.stt(h["st32"], h["st32"], last, pd, ALU.mult, ALU.add)
                    cnt[hh] += 1
                    k.copy(h["stbf"][cnt[hh] % 2], h["st32"], eng="act")
            for hh in range(2):
                h = H[hh]
                k.copy(h["o_sb"], ps_o[hh], eng=("act" if hh else "dve"))
                k.dma(oT[hh * 128:(hh + 1) * 128, t0:t0 + TT], h["o_sb"], q="sp", is_output=True)
    return k.finish()


MLA_SCALE = 192.0 ** -0.5
MASK_ENG = "dve"


def build_L4(B, S):
    k = K()
    T = B * S
    qn = k.dram("qn", [2, 128, T], BF16, "ExternalInput")
    qr = k.dram("qr", [2, 64, T], BF16, "ExternalInput")
    kn = k.dram("kn", [2, 128, T], BF16, "ExternalInput")
    kr = k.dram("kr", [64, T], BF16, "ExternalInput")
    vd = k.dram("v", [2, T, 128], BF16, "ExternalInput")
    maskd = k.dram("masks", [128, 4, 512], BF16, "ExternalInput")
    identd = k.dram("ident", [128, 128], BF16, "ExternalInput")
    oT = k.dram("oT", [2, T, 128], BF16, "ExternalOutput")
    cx = Ctx(k, identd)
    masks = k.sb("masks", [128, 4, 512], BF16)
    k.dma(masks, maskd)
    NKT = S // 128
    qn_sb = k.sb("qn_sb", [128, S], BF16)
    qr_sb = k.sb("qr_sb", [64, S], BF16)
    kn_sb = k.sb("kn_sb", [128, S], BF16)
    kr_sb = k.sb("kr_sb", [64, S], BF16)
    v_sb = k.sb("v_sb", [128, NKT, 136], BF16)
    k.memset(v_sb[:, :, 128:129], 1.0)
    ps_s = [k.ps(f"ps_s{i}") for i in range(3)]
    ps_o = [k.ps(f"ps_o{i}") for i in range(4)]
    pT = [k.sb(f"pT{i}", [128, 512], BF16) for i in range(4)]
    rden = k.sb("rden", [128, 4, 1], F32, units=4)
    on = k.sb("on", [128, 8, 128], BF16, units=8)
    o_sb = [k.sb(f"o_sb{i}", [128, 512], BF16) for i in range(2)]
    it = 0
    oi = 0
    for b in range(B):
        k.dma(kr_sb, kr[:, b * S:(b + 1) * S], q="sp")
        for hh in range(2):
            k.dma(qn_sb, qn[hh, :, b * S:(b + 1) * S], q="sp")
            k.dma(qr_sb, qr[hh, :, b * S:(b + 1) * S], q="act")
            k.dma(kn_sb, kn[hh, :, b * S:(b + 1) * S], q="sp")
            vv = vd[hh, b * S:(b + 1) * S, :].rearrange("(kt p) d -> p kt d", p=128)
            for k0 in range(0, NKT, 8):
                k.dma(v_sb[:, k0:k0 + 8, 0:128], vv[:, k0:k0 + 8, :], q=("act" if (k0 // 8) % 2 else "sp"))
            tiles = [(qi, kt) for qi in range(S // 512) for kt in range(4 * qi + 4)]
            LOOK = 2

            def emit_s(idx):
                qi, kt = tiles[idx]
                qs = slice(qi * 512, (qi + 1) * 512)
                ks = slice(kt * 128, (kt + 1) * 128)
                ps = ps_s[idx % 3]
                k.mm(ps, kn_sb[:, ks], qn_sb[:, qs], start=True, stop=False)
                k.mm(ps, kr_sb[:, ks], qr_sb[:, qs], start=False, stop=True)

            def emit_rest(idx):
                nonlocal oi
                qi, kt = tiles[idx]
                ps = ps_s[idx % 3]
                p = pT[idx % 4]
                k.act(p, ps, AF.Exp, scale=MLA_SCALE)
                j = kt - 4 * qi
                if j >= 0:
                    k.tt(p, p, masks[:, j, :], ALU.mult, eng=MASK_ENG)
                for sb in range(max(j, 0), 4):
                    k.mm(ps_o[sb][:, 0:129], p[:, sb * 128:(sb + 1) * 128], v_sb[:, kt, 0:129],
                         start=(kt == 0), stop=(kt == 4 * qi + sb))
                if kt == 4 * qi + 3:
                    for sb in range(4):
                        k.recip(rden[sb], ps_o[sb][:, 128:129])
                        o_ = on[oi % 8]
                        oi += 1
                        k.ts(o_, ps_o[sb][:, 0:128], rden[sb], ALU.mult)
                        r0 = b * S + qi * 512 + sb * 128
                        k.dma(oT[hh, r0:r0 + 128, :], o_, q="sp", is_output=True)

            for idx in range(len(tiles) + LOOK):
                if idx < len(tiles):
                    emit_s(idx)
                if idx - LOOK >= 0:
                    emit_rest(idx - LOOK)
    return k.finish()


class PsRot:
    def __init__(self, k, n, name="psr"):
        self.b = [k.ps(f"{name}{i}") for i in range(n)]
        self.i = 0

    def next(self):
        p = self.b[self.i % len(self.b)]
        self.i += 1
        return p


def linear(k, slabs, W, c0, nchunks, x_units, TT, psr, consume, width=128):
    KC = len(x_units)
    for j in range(nchunks):
        sl = slabs.load(W, c0 + j * width, width=width, kc=KC)
        ps = psr.next()
        for kc in range(KC):
            k.mm(ps[0:width, :TT], sl[:, kc, 0:width], x_units[kc][:, :TT], start=(kc == 0), stop=(kc == KC - 1))
        consume(j, ps)


def build_ffn(Tn, G=1024):
    k = K()
    uT = k.dram("uT", [D, Tn], BF16, "ExternalInput")
    wgu = k.dram("wgu", [D, 11264], F32, "ExternalInput")
    wdn = k.dram("wdn", [5632, D], F32, "ExternalInput")
    yT = k.dram("yT", [D, Tn], BF16, "ExternalOutput")
    FC = 44
    u_sb = k.sb("u_sb", [128, DC, G], BF16, units=DC)
    gated = k.sb("gated", [128, FC, G], BF16, units=FC)
    sl_gu = Slabs(k, "slgu", DC, 4)
    sl_d = Slabs(k, "sld", FC, 2)
    sgl = [k.sb(f"sgl{i}", [128, 512], BF16) for i in range(2)]
    ost = [k.sb(f"ost{i}", [128, 512], BF16) for i in range(3)]
    psr = PsRot(k, 8)
    n = 0
    for g in range(Tn // G):
        t0 = g * G
        for c in range(DC):
            k.dma(u_sb[c], uT[c * 128:(c + 1) * 128, t0:t0 + G], q=("sp" if c % 2 == 0 else "act"))
        for hb in range(FC):
            slg = sl_gu.load(wgu, hb * 128)
            slu = sl_gu.load(wgu, 5632 + hb * 128)
            for hf in range(G // 512):
                hs = slice(hf * 512, (hf + 1) * 512)
                pg = psr.next()
                pu = psr.next()
                for kc in range(DC):
                    k.mm(pg, slg[:, kc, :], u_sb[kc][:, hs], start=(kc == 0), stop=(kc == DC - 1))
                for kc in range(DC):
                    k.mm(pu, slu[:, kc, :], u_sb[kc][:, hs], start=(kc == 0), stop=(kc == DC - 1))
                s = sgl[n % 2]
                k.act(s, pg, AF.Silu)
                k.tt(gated[hb][:, hs], s, pu, ALU.mult)
                n += 1
        for dc in range(DC):
            sl = sl_d.load(wdn, dc * 128)
            for hf in range(G // 512):
                hs = slice(hf * 512, (hf + 1) * 512)
                ps = psr.next()
                for fc in range(FC):
                    k.mm(ps, sl[:, fc, :], gated[fc][:, hs], start=(fc == 0), stop=(fc == FC - 1))
                o = ost[n % 3]
                k.copy(o, ps, eng=("act" if n % 2 else "dve"))
                n += 1
                k.dma(yT[dc * 128:(dc + 1) * 128, t0 + hf * 512:t0 + (hf + 1) * 512], o, q="sp", is_output=True)
    return k.finish()


def build_post_mixer(Tc, hgrn, router, gate_c, shift_c, scale_c, TT=512):
    k = K()
    hT = k.dram("hT", [D, Tc], F32, "ExternalInput")
    oT = k.dram("oT", [D, Tc], BF16, "ExternalInput")
    W = k.dram("W", [D, D], F32, "ExternalInput")
    mods = k.dram("mods", [128, 224], F32, "ExternalInput")
    gains = k.dram("gains", [128, 3, DC], F32, "ExternalInput")
    houtT = k.dram("houtT", [D, Tc], F32, "ExternalOutput")
    uT = k.dram("uT", [D, Tc], BF16, "ExternalOutput")
    if hgrn:
        sgT = k.dram("sgT", [D, Tc], BF16, "ExternalInput")
    if router:
        wrd = k.dram("wr", [D, 8], F32, "ExternalInput")
        wd = k.dram("wd", [Tc, 8], F32, "ExternalOutput")
    cx = Ctx(k)
    m_sb = k.sb("m_sb", [128, 224], F32)
    g_sb = k.sb("g_sb", [128, 3, DC], F32)
    k.dma(m_sb, mods)
    k.dma(g_sb, gains)
    Gc = gate_coefs(k, "pm", m_sb, g_sb[:, 1, :], gate_c)
    A, B = mod_coefs(k, "pm", m_sb, g_sb[:, 2, :], shift_c, scale_c)
    o_sb = k.sb("o_sb", [128, DC, TT], BF16, units=DC)
    if hgrn:
        sg_sb = k.sb("sg_sb", [128, DC, TT], BF16, units=DC)
    sq = k.sb("sq", [128, DC, TT], BF16, units=DC)
    tmp = k.sb("tmp", [128, DC, TT], F32, units=DC)
    y_sb = k.sb("y_sb", [128, DC, TT], F32, units=DC)
    h_sb = k.sb("h_sb", [128, DC, TT], F32, units=DC)
    u_sb = k.sb("u_sb", [128, DC, TT], BF16, units=DC)
    rs = k.sb("rs", [128, TT], F32)
    slabs = Slabs(k, "slw", DC, 3)
    psr = PsRot(k, 6)
    ps_n = k.ps("ps_n")
    if router:
        wr_sb = k.sb("wr_sb", [128, DC, 8], F32)
        k.dma(wr_sb, wrd.rearrange("(kc p) e -> p kc e", p=128))
        ps_r = k.ps("ps_r", [128, 4, 8], F32, units=4)
        lg = k.sb("lg", [128, 4, 8], F32, units=4)
        top8 = k.sb("top8", [128, 4, 8], F32, units=4)
        msk = k.sb("msk", [128, 4, 8], F32, units=4)
        negm = k.sb("negm", [128, 4, 1], F32, units=4)
        ex = k.sb("ex", [128, 4, 8], F32, units=4)
        den = k.sb("den", [128, 4, 1], F32, units=4)
        wdt = k.sb("wdt", [128, 4, 8], F32, units=4)
    for t in range(Tc // TT):
        ts_ = slice(t * TT, (t + 1) * TT)
        for c in range(DC):
            k.dma(o_sb[c], oT[c * 128:(c + 1) * 128, ts_], q=("sp" if c % 2 == 0 else "act"))
        if hgrn:
            for c in range(DC):
                k.dma(sg_sb[c], sgT[c * 128:(c + 1) * 128, ts_], q=("sp" if c % 2 == 0 else "act"))
            cx.rstd(k, o_sb, TT, sq, ps_n, rs, D)
            for c in range(DC):
                k.tt(tmp[c], o_sb[c], rs, ALU.mult)
                k.stt(o_sb[c], tmp[c], g_sb[:, 0, c:c + 1], sg_sb[c], ALU.mult, ALU.mult)

        def cons(j, ps):
            k.copy(y_sb[j], ps, eng=("act" if j % 2 else "dve"))
        linear(k, slabs, W, 0, DC, o_sb, TT, psr, cons)
        cx.rstd(k, y_sb, TT, sq, ps_n, rs, D)
        for c in range(DC):
            k.dma(h_sb[c], hT[c * 128:(c + 1) * 128, ts_], q=("sp" if c % 2 == 0 else "act"))
        for c in range(DC):
            k.tt(tmp[c], y_sb[c], rs, ALU.mult)
            k.stt(h_sb[c], tmp[c], Gc[:, c:c + 1], h_sb[c], ALU.mult, ALU.add)
            k.dma(houtT[c * 128:(c + 1) * 128, ts_], h_sb[c], q="sp", is_output=True)
        cx.rstd(k, h_sb, TT, sq, ps_n, rs, D)
        norm_mod_apply(k, h_sb, rs, A, B, tmp, u_sb, TT, out2_units=(y_sb if router else None))
        for c in range(DC):
            k.dma(uT[c * 128:(c + 1) * 128, ts_], u_sb[c], q="sp", is_output=True)
        if router:
            for s4 in range(TT // 128):
                ss = slice(s4 * 128, (s4 + 1) * 128)
                for kc in range(DC):
                    k.mm(ps_r[s4], y_sb[kc][:, ss], wr_sb[:, kc, :], start=(kc == 0), stop=(kc == DC - 1))
                k.copy(lg[s4], ps_r[s4])
                a, b_ = top8[s4], lg[s4]
                k.generic("dve", lambda e, a=a, b_=b_: e.max(a.ap, b_.ap), [b_], [a])
                k.ts(msk[s4], lg[s4], top8[s4][:, 1:2], ALU.is_ge)
                k.ts(negm[s4], top8[s4][:, 0:1], -1.0, ALU.mult)
                k.act(ex[s4], lg[s4], AF.Exp, bias=negm[s4])
                k.tt(ex[s4], ex[s4], msk[s4], ALU.mult)
                d_, e_ = den[s4], ex[s4]
                k.generic("dve", lambda e, d_=d_, e_=e_: e.reduce_sum(d_.ap, e_.ap, mybir.AxisListType.X), [e_], [d_])
                k.recip(den[s4], den[s4])
                k.ts(wdt[s4], ex[s4], den[s4], ALU.mult)
                k.dma(wd[t * TT + s4 * 128:t * TT + (s4 + 1) * 128, :], wdt[s4], q="sp", is_output=True)
    return k.finish()

TWO_PI = 6.283185307179586
C1 = 6.28125
C2 = TWO_PI - C1


def build_post_ffn(Tc, moe, mla, gate_c, kv_cols=None, q_cols=None, TT=512):
    k = K()
    hT = k.dram("hT", [D, Tc], F32, "ExternalInput")
    mods = k.dram("mods", [128, 224], F32, "ExternalInput")
    gains = k.dram("gains", [128, 3, DC], F32, "ExternalInput")
    houtT = k.dram("houtT", [D, Tc], F32, "ExternalOutput")
    if moe:
        yaT = k.dram("yaT", [D, Tc], BF16, "ExternalInput")
        ybT = k.dram("ybT", [D, Tc], BF16, "ExternalInput")
        wab = k.dram("wab", [128, 2, Tc], F32, "ExternalInput")
    else:
        yT = k.dram("yT", [D, Tc], BF16, "ExternalInput")
    if mla:
        g4 = k.dram("g4", [128, 2, 4], F32, "ExternalInput")
        wkva = k.dram("wkva", [D, 576], F32, "ExternalInput")
        wkvb = k.dram("wkvb", [512, 4096], F32, "ExternalInput")
        wqa = k.dram("wqa", [D, 512], F32, "ExternalInput")
        wqb = k.dram("wqb", [512, 3072], F32, "ExternalInput")
        posd = k.dram("pos", [128, Tc], I32, "ExternalInput")
        invfd = k.dram("invf", [128, 1], F32, "ExternalInput")
        kvT = k.dram("kvT", [4096, Tc], BF16, "ExternalOutput")
        krT = k.dram("krT", [64, Tc], BF16, "ExternalOutput")
        qT = k.dram("qT", [3072, Tc], BF16, "ExternalOutput")
    cx = Ctx(k)
    m_sb = k.sb("m_sb", [128, 224], F32)
    g_sb = k.sb("g_sb", [128, 3, DC], F32)
    k.dma(m_sb, mods)
    k.dma(g_sb, gains)
    Gc = gate_coefs(k, "pf", m_sb, g_sb[:, 0, :], gate_c)
    h_sb = k.sb("h_sb", [128, DC, TT], F32, units=DC)
    sq = k.sb("sq", [128, DC, TT], BF16, units=DC)
    tmp = k.sb("tmp", [128, DC, TT], F32, units=DC)
    rs = k.sb("rs", [128, TT], F32)
    ps_n = k.ps("ps_n")
    if moe:
        ya_sb = k.sb("ya_sb", [128, DC, TT], BF16, units=DC)
        yb_sb = k.sb("yb_sb", [128, DC, TT], BF16, units=DC)
        y_sb = k.sb("y_sb", [128, DC, TT], F32, units=DC)
        w_sb = k.sb("w_sb", [128, 2, TT], F32)
    else:
        y_sb = k.sb("y_sb", [128, DC, TT], BF16, units=DC)
    if mla:
        g4_sb = k.sb("g4_sb", [128, 2, 4], F32)
        k.dma(g4_sb, g4)
        invf = k.sb("invf_sb", [128, 1], F32)
        k.dma(invf, invfd)
        Akv, Bkv = mod_coefs(k, "kv", m_sb, g_sb[:, 1, :], kv_cols[0], kv_cols[1])
        Aq, Bq = mod_coefs(k, "q", m_sb, g_sb[:, 2, :], q_cols[0], q_cols[1])
        un = k.sb("un", [128, DC, TT], BF16, units=DC)
        lat = k.sb("lat", [128, 4, TT], F32, units=4)
        latb = k.sb("latb", [128, 4, TT], BF16, units=4)
        kr12 = k.sb("kr12", [32, 2, TT], F32, units=2)
        qrp = k.sb("qrp", [128, 8, TT], F32, units=8)
        pos_i = k.sb("pos_i", [128, TT], I32)
        ang = k.sb("ang", [128, TT], F32)
        tq = k.sb("tq", [128, TT], F32)
        ki = k.sb("ki", [128, TT], I32)
        kf = k.sb("kf", [128, TT], F32)
        mk = k.sb("mk", [128, TT], F32)
        sin_t = k.sb("sin_t", [128, TT], F32)
        cos_t = k.sb("cos_t", [128, TT], F32)
        ra = k.sb("ra", [128, TT], F32)
        rb = k.sb("rb", [128, TT], F32)
        slabs = Slabs(k, "slw", DC, 3)
        slabs4 = Slabs(k, "slw4", 4, 3)
        psr = PsRot(k, 6)
        ost = [k.sb(f"ost{i}", [128, TT], BF16) for i in range(3)]
    n = 0
    for t in range(Tc // TT):
        ts_ = slice(t * TT, (t + 1) * TT)
        for c in range(DC):
            k.dma(h_sb[c], hT[c * 128:(c + 1) * 128, ts_], q=("sp" if c % 2 == 0 else "act"))
        if moe:
            k.dma(w_sb, wab[:, :, ts_])
            for c in range(DC):
                k.dma(ya_sb[c], yaT[c * 128:(c + 1) * 128, ts_], q="sp")
                k.dma(yb_sb[c], ybT[c * 128:(c + 1) * 128, ts_], q="act")
            for c in range(DC):
                k.tt(tmp[c], ya_sb[c], w_sb[:, 0, :], ALU.mult)
                k.tt(y_sb[c], yb_sb[c], w_sb[:, 1, :], ALU.mult)
                k.tt(y_sb[c], y_sb[c], tmp[c], ALU.add)
        else:
            for c in range(DC):
                k.dma(y_sb[c], yT[c * 128:(c + 1) * 128, ts_], q=("sp" if c % 2 == 1 else "act"))
        cx.rstd(k, y_sb, TT, sq, ps_n, rs, D)
        for c in range(DC):
            k.tt(tmp[c], y_sb[c], rs, ALU.mult)
            k.stt(h_sb[c], tmp[c], Gc[:, c:c + 1], h_sb[c], ALU.mult, ALU.add)
            k.dma(houtT[c * 128:(c + 1) * 128, ts_], h_sb[c], q="sp", is_output=True)
        if not mla:
            continue
        cx.rstd(k, h_sb, TT, sq, ps_n, rs, D)
        k.dma(pos_i, posd[:, ts_])
        k.copy(ang, pos_i)
        k.ts(ang, ang, invf[:, 0:1], ALU.mult)
        k.ts(tq, ang, 1.0 / TWO_PI, ALU.mult)
        k.copy(ki, tq)
        k.copy(kf, ki)
        k.stt(ang, kf, -C1, ang, ALU.mult, ALU.add)
        k.stt(ang, kf, -C2, ang, ALU.mult, ALU.add)
        k.ts(mk, ang, float(np.pi), ALU.is_gt)
        k.stt(ang, mk, -TWO_PI, ang, ALU.mult, ALU.add)
        k.ts(mk, ang, -float(np.pi), ALU.is_lt)
        k.stt(ang, mk, TWO_PI, ang, ALU.mult, ALU.add)
        k.act(sin_t, ang, AF.Sin)
        k.ts(tq, ang, float(np.pi / 2), ALU.add)
        k.ts(mk, tq, float(np.pi), ALU.is_gt)
        k.stt(tq, mk, -TWO_PI, tq, ALU.mult, ALU.add)
        k.act(cos_t, tq, AF.Sin)

        def rope(x1, x2, o1, o2, P):
            k.tt(ra[0:P], x1, cos_t[0:P], ALU.mult)
            k.tt(rb[0:P], x2, sin_t[0:P], ALU.mult)
            k.tt(o1, ra[0:P], rb[0:P], ALU.subtract)
            k.tt(ra[0:P], x2, cos_t[0:P], ALU.mult)
            k.tt(rb[0:P], x1, sin_t[0:P], ALU.mult)
            k.tt(o2, ra[0:P], rb[0:P], ALU.add)

        for c in range(DC):
            k.tt(tmp[c], h_sb[c], rs, ALU.mult)
            k.act(un[c], tmp[c], AF.Identity, bias=Bkv[:, c:c + 1], scale=Akv[:, c:c + 1])

        def cons_lat(j, ps):
            k.copy(lat[j], ps, eng=("act" if j % 2 else "dve"))
        linear(k, slabs, wkva, 0, 4, un, TT, psr, cons_lat)

        def cons_kr(j, ps):
            k.copy(kr12[j], ps[0:32, :], eng="act")
        linear(k, slabs, wkva, 512, 2, un, TT, psr, cons_kr, width=32)
        cx.rstd(k, lat, TT, sq, ps_n, rs2 := k_rs2(k), 512)
        for c in range(4):
            k.tt(lat[c], lat[c], rs2, ALU.mult)
            k.ts(latb[c], lat[c], g4_sb[:, 0, c:c + 1], ALU.mult)

        def cons_kv(j, ps):
            nonlocal n
            o = ost[n % 3]
            k.copy(o, ps, eng=("act" if n % 2 else "dve"))
            n += 1
            k.dma(kvT[j * 128:(j + 1) * 128, ts_], o, q="sp", is_output=True)
        linear(k, slabs4, wkvb, 0, 32, latb, TT, psr, cons_kv)
        o = ost[n % 3]
        n += 1
        rope(kr12[0], kr12[1], o[0:32], o[32:64], 32)
        k.dma(krT[:, ts_], o[0:64], q="sp", is_output=True)
        for c in range(DC):
            k.act(un[c], tmp[c], AF.Identity, bias=Bq[:, c:c + 1], scale=Aq[:, c:c + 1])
        linear(k, slabs, wqa, 0, 4, un, TT, psr, cons_lat)
        cx.rstd(k, lat, TT, sq, ps_n, rs2, 512)
        for c in range(4):
            k.tt(lat[c], lat[c], rs2, ALU.mult)
            k.ts(latb[c], lat[c], g4_sb[:, 1, c:c + 1], ALU.mult)

        def cons_q(j, ps):
            nonlocal n
            if j < 16:
                o = ost[n % 3]
                k.copy(o, ps, eng=("act" if n % 2 else "dve"))
                n += 1
                k.dma(qT[j * 128:(j + 1) * 128, ts_], o, q="sp", is_output=True)
            else:
                k.copy(qrp[j - 16], ps, eng=("act" if j % 2 else "dve"))
        linear(k, slabs4, wqb, 0, 24, latb, TT, psr, cons_q)
        for j in range(4):
            o1 = ost[n % 3]
            n += 1
            o2 = ost[n % 3]
            n += 1
            rope(qrp[j], qrp[4 + j], o1, o2, 128)
            k.dma(qT[2048 + j * 128:2048 + (j + 1) * 128, ts_], o1, q="sp", is_output=True)
            k.dma(qT[2560 + j * 128:2560 + (j + 1) * 128, ts_], o2, q="sp", is_output=True)
    return k.finish()


_rs2 = {}


def k_rs2(k):
    if id(k) not in _rs2:
        _rs2[id(k)] = k.sb("rs2", [128, 512], F32)
    return _rs2[id(k)]


import time as _time
import ml_dtypes
from concourse.bass_utils import run_bass_kernel_spmd

NCORES = 8
BF = ml_dtypes.bfloat16


def _fm(v):
    return np.ascontiguousarray(np.asarray(v, np.float32).reshape(-1, 128).T)


def _run(nc, in_maps, tag):
    t0 = _time.time()
    if os.environ.get("KPROF"):
        res = run_bass_kernel_spmd(nc, in_maps, core_ids=list(range(NCORES)), trace=True)
        print(f"[kernel] {tag}: exec_time_ns={res.exec_time_ns}", flush=True)
    else:
        res = run_bass_kernel_spmd(nc, in_maps, core_ids=list(range(NCORES)))
    print(f"[kernel] {tag}: {_time.time() - t0:.1f}s", flush=True)
    return res.results


def kernel(x, c, positions, ada_w, ada_b, norm_g, hg_w_in, hg_lb_logits, hg_out_norm_g,
           hg_w_out, kv_src_norm_g, kv_src_ada_w, kv_src_ada_b, mla_w_kv_a, mla_kv_norm_g,
           mla_w_kv_b, mla_w_q_a, mla_q_norm_g, mla_w_q_b, mla_w_o, ffn_w_gu, ffn_w_down,
           moe_w_router, moe_w_gu, moe_w_down):
    f32 = np.float32
    x = np.asarray(x, f32)
    Bn, S, _ = x.shape
    T = Bn * S
    Tc = T // NCORES
    cpb = NCORES // Bn
    A = lambda a: np.asarray(a)
    ada_w, ada_b, norm_g = A(ada_w), A(ada_b), A(norm_g)
    ident = np.eye(128, dtype=f32).astype(BF)

    W_all = np.concatenate([ada_w[0, 0], ada_w[0, 1], ada_w[1, 0], ada_w[1, 1], A(kv_src_ada_w)], axis=1)
    b_all = np.concatenate([ada_b[0, 0], ada_b[0, 1], ada_b[1, 0], ada_b[1, 1], A(kv_src_ada_b)])
    NCH = W_all.shape[1] // 128 // NCORES
    cT = np.ascontiguousarray(A(c).astype(f32).T.reshape(DC, 128, Bn).transpose(1, 0, 2))
    nc = build_L0(NCH)
    ims = [{"cT": cT, "W": np.ascontiguousarray(W_all[:, i * NCH * 128:(i + 1) * NCH * 128]),
            "bias": _fm(b_all[i * NCH * 128:(i + 1) * NCH * 128])} for i in range(NCORES)]
    r = _run(nc, ims, "L0 mods")
    del W_all
    mods_all = np.concatenate([r[i]["out"] for i in range(NCORES)], axis=1)
    mods_b = [np.ascontiguousarray(mods_all[:, :, b]) for b in range(Bn)]

    def tok(i):
        b = i // cpb
        s0 = (i % cpb) * Tc
        return b, s0

    xT = []
    for i in range(NCORES):
        b, s0 = tok(i)
        xT.append(np.ascontiguousarray(x[b, s0:s0 + Tc, :].T))

    nc = build_L1(Tc)
    ims = [{"xT": xT[i], "mods": mods_b[tok(i)[0]], "gains": _fm(norm_g[0, 0, 0])} for i in range(NCORES)]
    r = _run(nc, ims, "L1 norm0")
    u0T = np.concatenate([r[i]["uT"] for i in range(NCORES)], axis=1)

    w_in = A(hg_w_in)[0]
    lbl_all = A(hg_lb_logits).astype(f32)
    tri = np.triu(np.ones((64, 64), f32))
    rmask = np.ones((128, 512), f32)
    rmask[:, ::64] = 0
    nc = build_L2(Bn, S)
    ims = []
    for i in range(NCORES):
        hs = (2 * i, 2 * i + 1)
        blocks = []
        for base in (0, 2048, 4096, 6144):
            for h in hs:
                blocks.append(w_in[:, base + h * 128: base + (h + 1) * 128])
        lbl = np.stack([np.stack([lbl_all[0, h * 128:(h + 1) * 128], lbl_all[1, h * 128:(h + 1) * 128]], -1) for h in hs], 1)
        ims.append({"uT": u0T, "Wc": np.ascontiguousarray(np.concatenate(blocks, 1)), "lbl": np.ascontiguousarray(lbl.astype(f32)),
                    "ident": ident, "tri": tri, "rmask": rmask})
    r = _run(nc, ims, "L2 hgrn2")
    del u0T
    oT_all = np.concatenate([r[i]["oT"] for i in range(NCORES)], axis=0)
    sgT_all = np.concatenate([r[i]["sgT"] for i in range(NCORES)], axis=0)

    def cols(a, i):
        return np.ascontiguousarray(a[:, i * Tc:(i + 1) * Tc])

    nc = build_post_mixer(Tc, True, False, 32, 48, 64)
    g3 = np.ascontiguousarray(np.stack([_fm(A(hg_out_norm_g)[0]), _fm(norm_g[0, 0, 1]), _fm(norm_g[0, 1, 0])], 1))
    w_out = np.ascontiguousarray(A(hg_w_out)[0])
    ims = [{"hT": xT[i], "oT": cols(oT_all, i), "sgT": cols(sgT_all, i), "W": w_out, "mods": mods_b[tok(i)[0]], "gains": g3}
           for i in range(NCORES)]
    r = _run(nc, ims, "L3a post-hgrn2")
    del oT_all, sgT_all, xT
    h1T = [r[i]["houtT"] for i in range(NCORES)]
    u1T = [r[i]["uT"] for i in range(NCORES)]

    nc = build_ffn(Tc)
    wgu = np.ascontiguousarray(A(ffn_w_gu)[0])
    wdn = np.ascontiguousarray(A(ffn_w_down)[0])
    ims = [{"uT": u1T[i], "wgu": wgu, "wdn": wdn} for i in range(NCORES)]
    r = _run(nc, ims, "FFN dense")
    y2T = [r[i]["yT"] for i in range(NCORES)]
    del u1T, wgu, wdn

    nc = build_post_ffn(Tc, False, True, 80, kv_cols=(192, 208), q_cols=(96, 112))
    g3 = np.ascontiguousarray(np.stack([_fm(norm_g[0, 1, 1]), _fm(A(kv_src_norm_g)), _fm(norm_g[1, 0, 0])], 1))
    g4 = np.ascontiguousarray(np.stack([_fm(A(mla_kv_norm_g)), _fm(A(mla_q_norm_g)[0])], 1))
    perm = np.concatenate([[h * 192 + d for h in range(16) for d in range(128)],
                           [h * 192 + 128 + j for h in range(16) for j in range(32)],
                           [h * 192 + 160 + j for h in range(16) for j in range(32)]]).astype(np.int64)
    wqb = np.ascontiguousarray(A(mla_w_q_b)[0][:, perm])
    invf32 = (1.0 / (10000.0 ** (np.arange(0, 64, 2, dtype=f32) / 64))).astype(f32)
    invf = np.ascontiguousarray(np.concatenate([invf32] * 4).reshape(128, 1))
    pos = A(positions).astype(np.int32)
    ims = []
    for i in range(NCORES):
        b, s0 = tok(i)
        ims.append({"hT": h1T[i], "mods": mods_b[b], "gains": g3, "yT": y2T[i], "g4": g4,
                    "wkva": np.ascontiguousarray(A(mla_w_kv_a)), "wkvb": np.ascontiguousarray(A(mla_w_kv_b)),
                    "wqa": np.ascontiguousarray(A(mla_w_q_a)[0]), "wqb": wqb,
                    "pos": np.ascontiguousarray(np.broadcast_to(pos[b, s0:s0 + Tc][None], (128, Tc))), "invf": invf})
    r = _run(nc, ims, "L3c post-ffn + mla proj")
    del h1T, y2T
    h2T = [r[i]["houtT"] for i in range(NCORES)]
    kvT = np.concatenate([r[i]["kvT"] for i in range(NCORES)], axis=1)
    krT = np.ascontiguousarray(np.concatenate([r[i]["krT"] for i in range(NCORES)], axis=1))
    qT = np.concatenate([r[i]["qT"] for i in range(NCORES)], axis=1)

    kk = np.arange(128)[:, None, None]
    jj = np.arange(4)[None, :, None]
    qq = np.arange(512)[None, None, :]
    masks = ((kk + 128 * jj) <= qq).astype(f32).astype(BF)
    nc = build_L4(Bn, S)
    ims = []
    for i in range(NCORES):
        hs = (2 * i, 2 * i + 1)
        ims.append({
            "qn": np.ascontiguousarray(np.stack([qT[h * 128:(h + 1) * 128] for h in hs])),
            "qr": np.ascontiguousarray(np.stack([np.concatenate([qT[2048 + h * 32:2048 + (h + 1) * 32], qT[2560 + h * 32:2560 + (h + 1) * 32]], 0) for h in hs])),
            "kn": np.ascontiguousarray(np.stack([kvT[h * 256:h * 256 + 128] for h in hs])),
            "kr": krT,
            "v": np.ascontiguousarray(np.stack([kvT[h * 256 + 128:h * 256 + 256].T for h in hs])),
            "masks": masks, "ident": ident})
    r = _run(nc, ims, "L4 attention")
    del kvT, qT
    aT_all = np.concatenate([r[i]["oT"][hh].T for i in range(NCORES) for hh in range(2)], axis=0)

    nc = build_post_mixer(Tc, False, True, 128, 144, 160)
    g3 = np.ascontiguousarray(np.stack([_fm(np.ones(D, f32)), _fm(norm_g[1, 0, 1]), _fm(norm_g[1, 1, 0])], 1))
    w_o = np.ascontiguousarray(A(mla_w_o)[0])
    wr = np.ascontiguousarray(A(moe_w_router)[0].astype(f32))
    ims = [{"hT": h2T[i], "oT": cols(aT_all, i), "W": w_o, "mods": mods_b[tok(i)[0]], "gains": g3, "wr": wr} for i in range(NCORES)]
    r = _run(nc, ims, "L5 post-attn + router")
    del aT_all, h2T
    h3T = [r[i]["houtT"] for i in range(NCORES)]
    u3T = np.concatenate([r[i]["uT"] for i in range(NCORES)], axis=1)
    wd = np.concatenate([r[i]["wd"] for i in range(NCORES)], axis=0)

    sel = wd > 0
    NE = sel.shape[1]
    rank = np.cumsum(sel, axis=1)
    first = sel & (rank == 1)
    second = sel & (rank == 2)
    toks = [np.nonzero(first[:, e] | second[:, e])[0] for e in range(NE)]
    nmax = max(1, max(len(tk) for tk in toks))
    CAP = ((nmax + 1023) // 1024) * 1024
    print(f"[kernel] expert loads {[len(tk) for tk in toks]} CAP={CAP}", flush=True)
    nc = build_ffn(CAP)
    mgu, mdn = A(moe_w_gu)[0], A(moe_w_down)[0]
    ims = []
    for e in range(NE):
        ug = np.zeros((D, CAP), BF)
        ug[:, :len(toks[e])] = u3T[:, toks[e]]
        ims.append({"uT": ug, "wgu": np.ascontiguousarray(mgu[e]), "wdn": np.ascontiguousarray(mdn[e])})
    r = _run(nc, ims, "FFN experts")
    del u3T
    yaT = np.zeros((D, T), BF)
    ybT = np.zeros((D, T), BF)
    wa = np.zeros(T, f32)
    wb = np.zeros(T, f32)
    for e in range(NE):
        tk = toks[e]
        ye = r[e]["yT"][:, :len(tk)]
        f1 = first[tk, e]
        yaT[:, tk[f1]] = ye[:, f1]
        ybT[:, tk[~f1]] = ye[:, ~f1]
        wa[tk[f1]] = wd[tk[f1], e]
        wb[tk[~f1]] = wd[tk[~f1], e]

    nc = build_post_ffn(Tc, True, False, 176)
    g3 = np.ascontiguousarray(np.stack([_fm(norm_g[1, 1, 1]), _fm(np.ones(D, f32)), _fm(np.ones(D, f32))], 1))
    ims = []
    for i in range(NCORES):
        sl = slice(i * Tc, (i + 1) * Tc)
        wab = np.ascontiguousarray(np.broadcast_to(np.stack([wa[sl], wb[sl]])[None], (128, 2, Tc)))
        ims.append({"hT": h3T[i], "mods": mods_b[tok(i)[0]], "gains": g3, "yaT": cols(yaT, i), "ybT": cols(ybT, i), "wab": wab})
    r = _run(nc, ims, "L7 moe combine")
    out = np.empty((Bn, S, D), f32)
    for i in range(NCORES):
        b, s0 = tok(i)
        out[b, s0:s0 + Tc, :] = r[i]["houtT"].T
    return out
```

```python
import numpy as np
from contextlib import ExitStack
import concourse.bass as bass
import concourse.mybir as mybir

F32 = mybir.dt.float32
BF16 = mybir.dt.bfloat16
I32 = mybir.dt.int32
AF = mybir.ActivationFunctionType
ALU = mybir.AluOpType

ENGS = ("pe", "act", "dve", "pool", "sp")
import os
NDMASEM = 12


class Unit:
    __slots__ = ("name", "writer", "readers", "dreaders")

    def __init__(self, name):
        self.name = name
        self.writer = None
        self.readers = {}
        self.dreaders = []


class V:
    __slots__ = ("u", "ap")

    def __init__(self, u, ap):
        self.u = u
        self.ap = ap

    def __getitem__(self, idx):
        return V(self.u, self.ap[idx])


class Op:
    __slots__ = ("eng", "fn", "waits", "is_dma", "signal", "idx", "dsem", "dval", "cnt", "qi")

    def __init__(self, eng, fn, is_dma):
        self.eng = eng
        self.fn = fn
        self.is_dma = is_dma
        self.waits = []
        self.signal = False
        self.cnt = None


class K:
    def __init__(self, name="k"):
        self.nc = bass.Bass("TRN2", target_bir_lowering=False)
        self.es = ExitStack()
        self.ops = {e: [] for e in ENGS}
        self.ndma = {e: 0 for e in ENGS}
        self.dma_ops = {e: [] for e in ENGS}
        self.n = 0
        self.out_dmas = []

    def dram(self, name, shape, dt, kind):
        return self.nc.dram_tensor(name, list(shape), dt, kind=kind).ap()

    def sb(self, name, shape, dt=F32, units=1):
        t = self.es.enter_context(self.nc.sbuf_tensor("s_" + name, list(shape), dt))
        if units == 1:
            return V(Unit(name), t[:])
        return [V(Unit(f"{name}{j}"), t[:, j]) for j in range(units)]

    def ps(self, name, shape=(128, 512), dt=F32, units=1):
        t = self.es.enter_context(self.nc.psum_tensor("p_" + name, list(shape), dt))
        if units == 1:
            return V(Unit(name), t[:])
        return [V(Unit(f"{name}{j}"), t[:, j]) for j in range(units)]

    def _rec(self, eng, fn, reads, writes, is_dma=False):
        op = Op(eng, fn, is_dma)
        op.idx = len(self.ops[eng])
        deps = []
        for v in reads:
            u = v.u
            if u.writer is not None:
                deps.append((u.writer, "raw"))
        for v in writes:
            u = v.u
            if u.writer is not None:
                deps.append((u.writer, "waw"))
            for r in list(u.readers.values()) + u.dreaders:
                deps.append((r, "war"))
        seen = set()
        for p, kind in deps:
            if p is op or id(p) in seen:
                continue
            if (not p.is_dma) and p.eng == eng and not is_dma:
                if eng == "pe" or kind == "war":
                    continue
            seen.add(id(p))
            op.waits.append(p)
            p.signal = True
        for v in reads:
            if is_dma:
                v.u.dreaders.append(op)
            else:
                v.u.readers[eng] = op
        for v in writes:
            v.u.writer = op
            v.u.readers = {}
            v.u.dreaders = []
        if is_dma:
            op.qi = self.ndma[eng]
            self.ndma[eng] += 1
            self.dma_ops[eng].append(op)
            op.signal = True
        self.ops[eng].append(op)
        return op

    def eng_obj(self, e):
        nc = self.nc
        return {"pe": nc.tensor, "act": nc.scalar, "dve": nc.vector, "pool": nc.gpsimd, "sp": nc.sync}[e]

    def mm(self, out, lhsT, rhs, start=True, stop=True):
        return self._rec("pe", lambda e: e.matmul(out.ap, lhsT.ap, rhs.ap, start=start, stop=stop),
                         [lhsT, rhs] + ([] if start else [out]), [out])

    def transpose(self, out, in_, ident):
        return self._rec("pe", lambda e: e.transpose(out.ap, in_.ap, ident.ap), [in_, ident], [out])

    def act(self, out, in_, func, bias=None, scale=None, extra_reads=()):
        kw = {}
        rd = [in_] + list(extra_reads)
        if bias is not None:
            if isinstance(bias, V):
                kw["bias"] = bias.ap
                rd.append(bias)
            else:
                kw["bias"] = bias
        if scale is not None:
            if isinstance(scale, V):
                kw["scale"] = scale.ap
                rd.append(scale)
            else:
                kw["scale"] = scale
        return self._rec("act", lambda e: e.activation(out.ap, in_.ap, func, **kw), rd, [out])

    def tt(self, out, in0, in1, op, eng="dve"):
        return self._rec(eng, lambda e: e.tensor_tensor(out.ap, in0.ap, in1.ap, op), [in0, in1], [out])

    def ts(self, out, in0, s1, op0, s2=None, op1=None, eng="dve"):
        rd = [in0]
        a1 = s1
        a2 = s2
        if isinstance(s1, V):
            rd.append(s1)
            a1 = s1.ap
        if isinstance(s2, V):
            rd.append(s2)
            a2 = s2.ap
        if op1 is None:
            return self._rec(eng, lambda e: e.tensor_scalar(out.ap, in0.ap, a1, None, op0), rd, [out])
        return self._rec(eng, lambda e: e.tensor_scalar(out.ap, in0.ap, a1, a2, op0, op1), rd, [out])

    def stt(self, out, in0, scalar, in1, op0, op1):
        rd = [in0, in1]
        a = scalar
        if isinstance(scalar, V):
            rd.append(scalar)
            a = scalar.ap
        return self._rec("dve", lambda e: e.scalar_tensor_tensor(out.ap, in0.ap, a, in1.ap, op0, op1), rd, [out])

    def copy(self, out, in_, eng="dve"):
        if eng == "act":
            return self._rec("act", lambda e: e.copy(out.ap, in_.ap), [in_], [out])
        return self._rec(eng, lambda e: e.tensor_copy(out.ap, in_.ap), [in_], [out])

    def memset(self, out, val, eng="dve"):
        return self._rec(eng, lambda e: e.memset(out.ap, val), [], [out])

    def recip(self, out, in_):
        return self._rec("dve", lambda e: e.reciprocal(out.ap, in_.ap), [in_], [out])

    def generic(self, eng, fn, reads, writes):
        return self._rec(eng, fn, reads, writes)

    def dma(self, out, in_, q="sp", is_output=False):
        rd = [in_] if isinstance(in_, V) else []
        wr = [out] if isinstance(out, V) else []
        oa = out.ap if isinstance(out, V) else out
        ia = in_.ap if isinstance(in_, V) else in_
        op = self._rec(q, lambda e: e.dma_start(out=oa, in_=ia), rd, wr, is_dma=True)
        if is_output:
            self.out_dmas.append(op)
        return op

    def finish(self):
        nc = self.nc
        es = self.es
        csem = {e: es.enter_context(nc.semaphore(f"c_{e}")) for e in ENGS}
        dsem = {e: [es.enter_context(nc.semaphore(f"d_{e}{i}")) for i in range(NDMASEM)]
                for e in ENGS if self.ndma[e] > 0}
        for e in ENGS:
            c = 0
            for op in self.ops[e]:
                if op.is_dma:
                    op.dsem = dsem[e][op.qi % NDMASEM]
                    op.dval = 16 * (op.qi // NDMASEM + 1)
                elif op.signal:
                    c += 1
                    op.cnt = c
        final_waits = list(self.out_dmas)
        block = es.enter_context(nc.Block())

        def emit(e):
            eng = self.eng_obj(e)
            seen_c = {x: 0 for x in ENGS}
            seen_d = {}
            oplist = self.ops[e]

            def wait_on(p):
                if p.is_dma:
                    key = id(p.dsem)
                    if seen_d.get(key, 0) >= p.dval:
                        return
                    eng.wait_ge(p.dsem, p.dval)
                    seen_d[key] = p.dval
                else:
                    if seen_c[p.eng] >= p.cnt:
                        return
                    eng.wait_ge(csem[p.eng], p.cnt)
                    seen_c[p.eng] = p.cnt

            for op in oplist:
                for p in op.waits:
                    wait_on(p)
                if op.is_dma and op.qi >= NDMASEM:
                    wait_on(self.dma_ops[e][op.qi - NDMASEM])
                ins = op.fn(eng)
                if op.is_dma:
                    ins.then_inc(op.dsem, 16)
                elif op.signal:
                    ins.then_inc(csem[e], 1)
            if e == "sp":
                for p in final_waits:
                    wait_on(p)

        block.sync(lambda s: emit("sp"))
        block.tensor(lambda s: emit("pe"))
        block.scalar(lambda s: emit("act"))
        block.vector(lambda s: emit("dve"))
        block.gpsimd(lambda s: emit("pool"))
        es.close()
        return nc


D = 2048
DC = 16
EPS = 1e-6


def chunked(ap, p=128):
    return ap.rearrange("(c p) t -> p c t", p=p)


class Ctx:
    def __init__(self, k, ident_dram=None):
        self.k = k
        self.ones = k.sb("ones_bf", [128, 128], BF16)
        k.memset(self.ones, 1.0)
        self.eps = k.sb("eps_t", [128, 1], F32)
        k.memset(self.eps, EPS)
        if ident_dram is not None:
            self.ident = k.sb("ident_bf", [128, 128], BF16)
            k.dma(self.ident, ident_dram, q="pool")
        self.pi = 0

    def rstd(self, k, x_units, TT, sq_units, ps, out, nfeat):
        n = len(x_units)
        for c in range(n):
            k.act(sq_units[c][:, :TT], x_units[c][:, :TT], AF.Square)
        for c in range(n):
            k.mm(ps[:, :TT], self.ones, sq_units[c][:, :TT], start=(c == 0), stop=(c == n - 1))
        k.act(out[:, :TT], ps[:, :TT], AF.Sqrt, bias=self.eps, scale=1.0 / nfeat)
        k.recip(out[:, :TT], out[:, :TT])


def mod_coefs(k, name, mods, g, shift_c, scale_c, n=DC):
    A = k.sb(name + "_A", [128, n], F32)
    k.stt(A, mods[:, scale_c:scale_c + n], 1.0, g, ALU.add, ALU.mult)
    return A, mods[:, shift_c:shift_c + n]


def gate_coefs(k, name, mods, g, gate_c, n=DC):
    G = k.sb(name + "_G", [128, n], F32)
    k.tt(G, mods[:, gate_c:gate_c + n], g, ALU.mult)
    return G


def norm_mod_apply(k, x_units, rstd, A, B, tmp_units, out_units, TT, out2_units=None):
    n = len(x_units)
    for c in range(n):
        k.tt(tmp_units[c][:, :TT], x_units[c][:, :TT], rstd[:, :TT], ALU.mult)
        k.act(out_units[c][:, :TT], tmp_units[c][:, :TT], AF.Identity, bias=B[:, c:c + 1], scale=A[:, c:c + 1])
        if out2_units is not None:
            k.ts(out2_units[c][:, :TT], tmp_units[c][:, :TT], A[:, c:c + 1], ALU.mult, B[:, c:c + 1], ALU.add)


class Slabs:
    def __init__(self, k, name, KC, nbuf, width=128):
        self.k = k
        self.bufs = [k.sb(f"{name}{i}", [128, KC, width], BF16) for i in range(nbuf)]
        self.i = 0
        self.KC = KC

    def load(self, W, c0, width=128, kc=None):
        b = self.bufs[self.i % len(self.bufs)]
        self.i += 1
        kc = kc or self.KC
        self.k.dma(b[:, :kc, :width], W.rearrange("(kc p) n -> p kc n", p=128)[:, :, c0:c0 + width], q="pool")
        return b


def build_L0(NCH=28):
    k = K()
    cT = k.dram("cT", [128, DC, 2], F32, "ExternalInput")
    W = k.dram("W", [D, NCH * 128], F32, "ExternalInput")
    bias = k.dram("bias", [128, NCH], F32, "ExternalInput")
    out = k.dram("out", [128, NCH, 2], F32, "ExternalOutput")
    c_sb = k.sb("c_sb", [128, DC, 2], F32)
    sc = k.sb("sc", [128, DC, 2], F32)
    b_sb = k.sb("b_sb", [128, NCH], F32)
    o_sb = k.sb("o_sb", [128, NCH, 2], F32)
    slabs = [k.sb(f"sl{i}", [128, DC, 128], F32) for i in range(4)]
    pss = [k.ps(f"ps{i}") for i in range(4)]
    k.dma(c_sb, cT)
    k.dma(b_sb, bias)
    k.act(sc, c_sb, AF.Silu)
    Wv = W.rearrange("(kc p) n -> p kc n", p=128)
    for j in range(NCH):
        sl = slabs[j % 4]
        k.dma(sl, Wv[:, :, j * 128:(j + 1) * 128], q=("sp" if j % 2 == 0 else "act"))
        ps = pss[j % 4]
        for kc in range(DC):
            k.mm(ps[:, 0:2], sl[:, kc, :], sc[:, kc, :], start=(kc == 0), stop=(kc == DC - 1))
        k.ts(o_sb[:, j, :], ps[:, 0:2], b_sb[:, j:j + 1], ALU.add)
    k.dma(out, o_sb, is_output=True)
    return k.finish()


def build_L1(Tc, TT=512):
    k = K()
    xT = k.dram("xT", [D, Tc], F32, "ExternalInput")
    mods = k.dram("mods", [128, 224], F32, "ExternalInput")
    gains = k.dram("gains", [128, DC], F32, "ExternalInput")
    uT = k.dram("uT", [D, Tc], BF16, "ExternalOutput")
    cx = Ctx(k)
    m_sb = k.sb("m_sb", [128, 224], F32)
    g_sb = k.sb("g_sb", [128, DC], F32)
    k.dma(m_sb, mods)
    k.dma(g_sb, gains)
    A, B = mod_coefs(k, "n0", m_sb, g_sb, 0, 16)
    NB = 2
    xs = [k.sb(f"x{b}", [128, DC, TT], F32, units=DC) for b in range(NB)]
    sq = k.sb("sq", [128, DC, TT], BF16, units=DC)
    tmp = k.sb("tmp", [128, DC, TT], F32, units=DC)
    us = [k.sb(f"u{b}", [128, DC, TT], BF16, units=DC) for b in range(NB)]
    rs = [k.sb(f"rs{b}", [128, TT], F32) for b in range(NB)]
    pss = [k.ps(f"ps{i}") for i in range(2)]
    xv = chunked(xT)
    uv = chunked(uT)
    for t in range(Tc // TT):
        b = t % NB
        for c in range(DC):
            k.dma(xs[b][c], xv[:, c, t * TT:(t + 1) * TT], q=("sp" if c % 2 == 0 else "act"))
        cx.rstd(k, xs[b], TT, sq, pss[b], rs[b], D)
        norm_mod_apply(k, xs[b], rs[b], A, B, tmp, us[b], TT)
        for c in range(DC):
            k.dma(uv[:, c, t * TT:(t + 1) * TT], us[b][c], q="sp", is_output=True)
    return k.finish()


def build_L2(B, S, TT=512):
    k = K()
    T = B * S
    uT = k.dram("uT", [D, T], BF16, "ExternalInput")
    Wc = k.dram("Wc", [D, 1024], F32, "ExternalInput")
    lbl = k.dram("lbl", [128, 2, 2], F32, "ExternalInput")
    identd = k.dram("ident", [128, 128], BF16, "ExternalInput")
    trid = k.dram("tri", [64, 64], F32, "ExternalInput")
    rmaskd = k.dram("rmask", [128, TT], F32, "ExternalInput")
    oT = k.dram("oT", [256, T], BF16, "ExternalOutput")
    sgT = k.dram("sgT", [256, T], BF16, "ExternalOutput")
    cx = Ctx(k, identd)
    tri = k.sb("tri", [64, 64], F32)
    k.dma(tri, trid)
    rmask = k.sb("rmask", [128, TT], F32)
    k.dma(rmask, rmaskd)
    w_sb = k.sb("w_sb", [128, DC, 1024], BF16)
    for kc in range(DC):
        k.dma(w_sb[:, kc, :], Wc[kc * 128:(kc + 1) * 128, :], q="pool")
    lb_in = k.sb("lb_in", [128, 2, 2], F32)
    k.dma(lb_in, lbl)
    lb = k.sb("lb", [128, 2], F32)
    oml = k.sb("oml", [128, 2], F32)
    k.tt(lb, lb_in[:, :, 0], lb_in[:, :, 1], ALU.subtract)
    k.act(lb, lb, AF.Sigmoid)
    k.ts(oml, lb, -1.0, ALU.mult, 1.0, ALU.add)
    NCK = TT // 64
    u_sb = [k.sb(f"u{i}", [128, DC, TT], BF16, units=DC) for i in range(2)]
    ps_p = [k.ps(f"ps_p{i}") for i in range(3)]
    ps_o = [k.ps(f"ps_o{i}") for i in range(2)]
    ps_s = k.ps("ps_s", [64, 8, 64], F32, units=8)
    ps_d = k.ps("ps_d", [128, 4, 128], F32, units=4)
    ps_t = k.ps("ps_t", [128, 4, 256], BF16, units=4)
    H = []
    for hh in range(2):
        h = dict(
            fval=k.sb(f"fval{hh}", [128, TT], F32), logf=k.sb(f"logf{hh}", [128, TT], F32),
            kval=k.sb(f"kval{hh}", [128, TT], F32), bcum=k.sb(f"bcum{hh}", [128, TT], F32),
            eb=k.sb(f"eb{hh}", [128, TT], F32), enb=k.sb(f"enb{hh}", [128, TT], F32),
            q32=k.sb(f"q32{hh}", [128, TT], F32),
            qtil=k.sb(f"qtil{hh}", [128, TT], BF16), ktil=k.sb(f"ktil{hh}", [128, TT], BF16),
            khT=k.sb(f"khT{hh}", [128, NCK, 64], BF16, units=NCK),
            iT=k.sb(f"iT{hh}", [128, TT], BF16), sg=k.sb(f"sg{hh}", [128, TT], BF16),
            tok=k.sb(f"tok{hh}", [64, 4, 256], BF16, units=4),
            sT=k.sb(f"sT{hh}", [64, 4, 64], BF16, units=4),
            st32=k.sb(f"st32{hh}", [128, 128], F32),
            stbf=k.sb(f"stbf{hh}", [128, 2, 128], BF16, units=2),
            o_sb=k.sb(f"o_sb{hh}", [128, TT], BF16),
        )
        H.append(h)
    cnt = [0, 0]
    ti = 0
    pn = 0
    for b in range(B):
        for hh in range(2):
            k.memset(H[hh]["st32"], 0.0)
            k.memset(H[hh]["stbf"][cnt[hh] % 2], 0.0)
        for t in range(S // TT):
            t0 = b * S + t * TT
            u = u_sb[ti % 2]
            ti += 1
            for kc in range(DC):
                k.dma(u[kc], uT[kc * 128:(kc + 1) * 128, t0:t0 + TT], q=("sp" if kc % 2 == 0 else "act"))
            for hh in range(2):
                h = H[hh]
                for (kind, blk) in (("f", 2 + hh), ("q", hh), ("i", 4 + hh), ("g", 6 + hh)):
                    ps = ps_p[pn % 3]
                    pn += 1
                    for kc in range(DC):
                        k.mm(ps, w_sb[:, kc, blk * 128:(blk + 1) * 128], u[kc], start=(kc == 0), stop=(kc == DC - 1))
                    if kind == "f":
                        k.act(h["fval"], ps, AF.Sigmoid)
                    elif kind == "q":
                        k.copy(h["q32"], ps, eng="dve")
                    elif kind == "i":
                        k.copy(h["iT"], ps, eng="act")
                    else:
                        k.act(h["sg"], ps, AF.Silu)
                        k.dma(sgT[hh * 128:(hh + 1) * 128, t0:t0 + TT], h["sg"], q="sp", is_output=True)
            for hh in range(2):
                h = H[hh]
                k.ts(h["fval"], h["fval"], oml[:, hh:hh + 1], ALU.mult, lb[:, hh:hh + 1], ALU.add)
                k.act(h["logf"], h["fval"], AF.Ln)
                k.ts(h["kval"], h["fval"], -1.0, ALU.mult, 1.0, ALU.add)
                bc, lf = h["bcum"], h["logf"]
                k.generic("dve", lambda e, bc=bc, lf=lf: e.tensor_tensor_scan(bc.ap, rmask.ap, lf.ap, 0.0, ALU.mult, ALU.add),
                          [rmask, lf], [bc])
                k.act(h["eb"], h["bcum"], AF.Exp)
                k.act(h["enb"], h["bcum"], AF.Exp, scale=-1.0)
                k.tt(h["qtil"], h["q32"], h["eb"], ALU.mult)
                k.tt(h["ktil"], h["kval"], h["enb"], ALU.mult)
            for c in range(NCK):
                cs = slice(c * 64, (c + 1) * 64)
                for hh in range(2):
                    h = H[hh]
                    ui = 2 * c + hh
                    last = h["eb"][:, c * 64 + 63:c * 64 + 64]
                    k.ts(h["khT"][c], h["ktil"][:, cs], last, ALU.mult)
                    pt = ps_t[ui % 4]
                    k.transpose(pt[0:64, 0:128], h["khT"][c], cx.ident)
                    k.transpose(pt[0:64, 128:256], h["iT"][:, cs], cx.ident)
                    tok = h["tok"][c % 4]
                    k.copy(tok, pt[0:64, :], eng="act")
                    pss = ps_s[ui % 8]
                    k.mm(pss, h["ktil"][:, cs], h["qtil"][:, cs])
                    sT = h["sT"][c % 4]
                    k.tt(sT, pss, tri, ALU.mult)
                    sb_cur = h["stbf"][cnt[hh] % 2]
                    po = ps_o[hh]
                    k.mm(po[:, cs], sb_cur, h["qtil"][:, cs], start=True, stop=False)
                    k.mm(po[:, cs], tok[:, 128:256], sT, start=False, stop=True)
                    pd = ps_d[ui % 4]
                    k.mm(pd, tok[:, 0:128], tok[:, 128:256])
                    k.stt(h["st32"], h["st32"], last, pd, ALU.mult, ALU.add)
                    cnt[hh] += 1
                    k.copy(h["stbf"][cnt[hh] % 2], h["st32"], eng="act")
            for hh in range(2):
                h = H[hh]
                k.copy(h["o_sb"], ps_o[hh], eng=("act" if hh else "dve"))
                k.dma(oT[hh * 128:(hh + 1) * 128, t0:t0 + TT], h["o_sb"], q="sp", is_output=True)
    return k.finish()


MLA_SCALE = 192.0 ** -0.5
MASK_ENG = "dve"


def build_L4(B, S):
    k = K()
    T = B * S
    qn = k.dram("qn", [2, 128, T], BF16, "ExternalInput")
    qr = k.dram("qr", [2, 64, T], BF16, "ExternalInput")
    kn = k.dram("kn", [2, 128, T], BF16, "ExternalInput")
    kr = k.dram("kr", [64, T], BF16, "ExternalInput")
    vd = k.dram("v", [2, T, 128], BF16, "ExternalInput")
    maskd = k.dram("masks", [128, 4, 512], BF16, "ExternalInput")
    identd = k.dram("ident", [128, 128], BF16, "ExternalInput")
    oT = k.dram("oT", [2, T, 128], BF16, "ExternalOutput")
    cx = Ctx(k, identd)
    masks = k.sb("masks", [128, 4, 512], BF16)
    k.dma(masks, maskd)
    NKT = S // 128
    qn_sb = k.sb("qn_sb", [128, S], BF16)
    qr_sb = k.sb("qr_sb", [128, S], BF16)
    kn_sb = k.sb("kn_sb", [128, S], BF16)
    kr_sb = k.sb("kr_sb", [128, S], BF16)
    k.memset(qr_sb[64:128, :], 0.0)
    k.memset(kr_sb[64:128, :], 0.0, eng="pool")
    v_sb = k.sb("v_sb", [128, NKT, 136], BF16)
    k.memset(v_sb[:, :, 128:129], 1.0)
    ps_s = [k.ps(f"ps_s{i}") for i in range(3)]
    ps_o = [k.ps(f"ps_o{i}") for i in range(4)]
    pT = [k.sb(f"pT{i}", [128, 512], BF16) for i in range(4)]
    rden = k.sb("rden", [128, 4, 1], F32, units=4)
    on = k.sb("on", [128, 8, 128], BF16, units=8)
    o_sb = [k.sb(f"o_sb{i}", [128, 512], BF16) for i in range(2)]
    it = 0
    oi = 0
    for b in range(B):
        k.dma(kr_sb[0:64, :], kr[:, b * S:(b + 1) * S], q="sp")
        for hh in range(2):
            k.dma(qn_sb, qn[hh, :, b * S:(b + 1) * S], q="sp")
            k.dma(qr_sb[0:64, :], qr[hh, :, b * S:(b + 1) * S], q="act")
            k.dma(kn_sb, kn[hh, :, b * S:(b + 1) * S], q="sp")
            vv = vd[hh, b * S:(b + 1) * S, :].rearrange("(kt p) d -> p kt d", p=128)
            for k0 in range(0, NKT, 8):
                k.dma(v_sb[:, k0:k0 + 8, 0:128], vv[:, k0:k0 + 8, :], q=("act" if (k0 // 8) % 2 else "sp"))
            tiles = [(qi, kt) for qi in range(S // 512) for kt in range(4 * qi + 4)]
            LOOK = 2

            def emit_s(idx):
                qi, kt = tiles[idx]
                qs = slice(qi * 512, (qi + 1) * 512)
                ks = slice(kt * 128, (kt + 1) * 128)
                ps = ps_s[idx % 3]
                k.mm(ps, kn_sb[:, ks], qn_sb[:, qs], start=True, stop=False)
                k.mm(ps, kr_sb[:, ks], qr_sb[:, qs], start=False, stop=True)

            def emit_rest(idx):
                nonlocal oi
                qi, kt = tiles[idx]
                ps = ps_s[idx % 3]
                p = pT[idx % 4]
                k.act(p, ps, AF.Exp, scale=MLA_SCALE)
                j = kt - 4 * qi
                if j >= 0:
                    k.tt(p, p, masks[:, j, :], ALU.mult, eng=MASK_ENG)
                for sb in range(max(j, 0), 4):
                    k.mm(ps_o[sb][:, 0:129], p[:, sb * 128:(sb + 1) * 128], v_sb[:, kt, 0:129],
                         start=(kt == 0), stop=(kt == 4 * qi + sb))
                if kt == 4 * qi + 3:
                    for sb in range(4):
                        k.recip(rden[sb], ps_o[sb][:, 128:129])
                        o_ = on[oi % 8]
                        oi += 1
                        k.ts(o_, ps_o[sb][:, 0:128], rden[sb], ALU.mult)
                        r0 = b * S + qi * 512 + sb * 128
                        k.dma(oT[hh, r0:r0 + 128, :], o_, q="sp", is_output=True)

            for idx in range(len(tiles) + LOOK):
                if idx < len(tiles):
                    emit_s(idx)
                if idx - LOOK >= 0:
                    emit_rest(idx - LOOK)
    return k.finish()


class PsRot:
    def __init__(self, k, n, name="psr"):
        self.b = [k.ps(f"{name}{i}") for i in range(n)]
        self.i = 0

    def next(self):
        p = self.b[self.i % len(self.b)]
        self.i += 1
        return p


def linear(k, slabs, W, c0, nchunks, x_units, TT, psr, consume, width=128):
    KC = len(x_units)
    for j in range(nchunks):
        sl = slabs.load(W, c0 + j * width, width=width, kc=KC)
        ps = psr.next()
        for kc in range(KC):
            k.mm(ps[0:width, :TT], sl[:, kc, 0:width], x_units[kc][:, :TT], start=(kc == 0), stop=(kc == KC - 1))
        consume(j, ps)


def build_ffn(Tn, G=1024):
    k = K()
    uT = k.dram("uT", [D, Tn], BF16, "ExternalInput")
    wgu = k.dram("wgu", [D, 11264], F32, "ExternalInput")
    wdn = k.dram("wdn", [5632, D], F32, "ExternalInput")
    yT = k.dram("yT", [D, Tn], BF16, "ExternalOutput")
    FC = 44
    u_sb = k.sb("u_sb", [128, DC, G], BF16, units=DC)
    gated = k.sb("gated", [128, FC, G], BF16, units=FC)
    sl_gu = Slabs(k, "slgu", DC, 4)
    sl_d = Slabs(k, "sld", FC, 2)
    sgl = [k.sb(f"sgl{i}", [128, 512], BF16) for i in range(2)]
    ost = [k.sb(f"ost{i}", [128, 512], BF16) for i in range(3)]
    psr = PsRot(k, 8)
    n = 0
    widths = [G] * (Tn // G) + ([Tn % G] if Tn % G else [])
    t0 = -G
    for gw in widths:
        t0 += G
        for c in range(DC):
            k.dma(u_sb[c][:, :gw], uT[c * 128:(c + 1) * 128, t0:t0 + gw], q=("sp" if c % 2 == 0 else "act"))
        for hb in range(FC):
            slg = sl_gu.load(wgu, hb * 128)
            slu = sl_gu.load(wgu, 5632 + hb * 128)
            for hf in range(gw // 512):
                hs = slice(hf * 512, (hf + 1) * 512)
                pg = psr.next()
                pu = psr.next()
                for kc in range(DC):
                    k.mm(pg, slg[:, kc, :], u_sb[kc][:, hs], start=(kc == 0), stop=(kc == DC - 1))
                for kc in range(DC):
                    k.mm(pu, slu[:, kc, :], u_sb[kc][:, hs], start=(kc == 0), stop=(kc == DC - 1))
                s = sgl[n % 2]
                k.act(s, pg, AF.Silu)
                k.tt(gated[hb][:, hs], s, pu, ALU.mult)
                n += 1
        for dc in range(DC):
            sl = sl_d.load(wdn, dc * 128)
            for hf in range(gw // 512):
                hs = slice(hf * 512, (hf + 1) * 512)
                ps = psr.next()
                for fc in range(FC):
                    k.mm(ps, sl[:, fc, :], gated[fc][:, hs], start=(fc == 0), stop=(fc == FC - 1))
                o = ost[n % 3]
                k.copy(o, ps, eng=("act" if n % 2 else "dve"))
                n += 1
                k.dma(yT[dc * 128:(dc + 1) * 128, t0 + hf * 512:t0 + (hf + 1) * 512], o, q="sp", is_output=True)
    return k.finish()


def build_post_mixer(Tc, hgrn, router, gate_c, shift_c, scale_c, TT=512):
    k = K()
    hT = k.dram("hT", [D, Tc], F32, "ExternalInput")
    oT = k.dram("oT", [D, Tc], BF16, "ExternalInput")
    W = k.dram("W", [D, D], F32, "ExternalInput")
    mods = k.dram("mods", [128, 224], F32, "ExternalInput")
    gains = k.dram("gains", [128, 3, DC], F32, "ExternalInput")
    houtT = k.dram("houtT", [D, Tc], F32, "ExternalOutput")
    uT = k.dram("uT", [D, Tc], BF16, "ExternalOutput")
    if hgrn:
        sgT = k.dram("sgT", [D, Tc], BF16, "ExternalInput")
    if router:
        wrd = k.dram("wr", [D, 8], F32, "ExternalInput")
        wd = k.dram("wd", [Tc, 8], F32, "ExternalOutput")
    cx = Ctx(k)
    m_sb = k.sb("m_sb", [128, 224], F32)
    g_sb = k.sb("g_sb", [128, 3, DC], F32)
    k.dma(m_sb, mods)
    k.dma(g_sb, gains)
    Gc = gate_coefs(k, "pm", m_sb, g_sb[:, 1, :], gate_c)
    A, B = mod_coefs(k, "pm", m_sb, g_sb[:, 2, :], shift_c, scale_c)
    o_sb = k.sb("o_sb", [128, DC, TT], BF16, units=DC)
    if hgrn:
        sg_sb = k.sb("sg_sb", [128, DC, TT], BF16, units=DC)
    sq = k.sb("sq", [128, DC, TT], BF16, units=DC)
    tmp = k.sb("tmp", [128, DC, TT], F32, units=DC)
    y_sb = k.sb("y_sb", [128, DC, TT], F32, units=DC)
    h_sb = k.sb("h_sb", [128, DC, TT], F32, units=DC)
    u_sb = k.sb("u_sb", [128, DC, TT], BF16, units=DC)
    rs = k.sb("rs", [128, TT], F32)
    slabs = Slabs(k, "slw", DC, 3)
    psr = PsRot(k, 6)
    ps_n = k.ps("ps_n")
    if router:
        wr_sb = k.sb("wr_sb", [128, DC, 8], F32)
        k.dma(wr_sb, wrd.rearrange("(kc p) e -> p kc e", p=128))
        ps_r = k.ps("ps_r", [128, 4, 8], F32, units=4)
        lg = k.sb("lg", [128, 4, 8], F32, units=4)
        top8 = k.sb("top8", [128, 4, 8], F32, units=4)
        msk = k.sb("msk", [128, 4, 8], F32, units=4)
        negm = k.sb("negm", [128, 4, 1], F32, units=4)
        ex = k.sb("ex", [128, 4, 8], F32, units=4)
        den = k.sb("den", [128, 4, 1], F32, units=4)
        wdt = k.sb("wdt", [128, 4, 8], F32, units=4)
    for t in range(Tc // TT):
        ts_ = slice(t * TT, (t + 1) * TT)
        for c in range(DC):
            k.dma(o_sb[c], oT[c * 128:(c + 1) * 128, ts_], q=("sp" if c % 2 == 0 else "act"))
        if hgrn:
            for c in range(DC):
                k.dma(sg_sb[c], sgT[c * 128:(c + 1) * 128, ts_], q=("sp" if c % 2 == 0 else "act"))
            cx.rstd(k, o_sb, TT, sq, ps_n, rs, D)
            for c in range(DC):
                k.tt(tmp[c], o_sb[c], rs, ALU.mult)
                k.stt(o_sb[c], tmp[c], g_sb[:, 0, c:c + 1], sg_sb[c], ALU.mult, ALU.mult)

        def cons(j, ps):
            k.copy(y_sb[j], ps, eng=("act" if j % 2 else "dve"))
        linear(k, slabs, W, 0, DC, o_sb, TT, psr, cons)
        cx.rstd(k, y_sb, TT, sq, ps_n, rs, D)
        for c in range(DC):
            k.dma(h_sb[c], hT[c * 128:(c + 1) * 128, ts_], q=("sp" if c % 2 == 0 else "act"))
        for c in range(DC):
            k.tt(tmp[c], y_sb[c], rs, ALU.mult)
            k.stt(h_sb[c], tmp[c], Gc[:, c:c + 1], h_sb[c], ALU.mult, ALU.add)
            k.dma(houtT[c * 128:(c + 1) * 128, ts_], h_sb[c], q="sp", is_output=True)
        cx.rstd(k, h_sb, TT, sq, ps_n, rs, D)
        norm_mod_apply(k, h_sb, rs, A, B, tmp, u_sb, TT, out2_units=(y_sb if router else None))
        for c in range(DC):
            k.dma(uT[c * 128:(c + 1) * 128, ts_], u_sb[c], q="sp", is_output=True)
        if router:
            for s4 in range(TT // 128):
                ss = slice(s4 * 128, (s4 + 1) * 128)
                for kc in range(DC):
                    k.mm(ps_r[s4], y_sb[kc][:, ss], wr_sb[:, kc, :], start=(kc == 0), stop=(kc == DC - 1))
                k.copy(lg[s4], ps_r[s4])
                a, b_ = top8[s4], lg[s4]
                k.generic("dve", lambda e, a=a, b_=b_: e.max(a.ap, b_.ap), [b_], [a])
                k.ts(msk[s4], lg[s4], top8[s4][:, 1:2], ALU.is_ge)
                k.ts(negm[s4], top8[s4][:, 0:1], -1.0, ALU.mult)
                k.act(ex[s4], lg[s4], AF.Exp, bias=negm[s4])
                k.tt(ex[s4], ex[s4], msk[s4], ALU.mult)
                d_, e_ = den[s4], ex[s4]
                k.generic("dve", lambda e, d_=d_, e_=e_: e.reduce_sum(d_.ap, e_.ap, mybir.AxisListType.X), [e_], [d_])
                k.recip(den[s4], den[s4])
                k.ts(wdt[s4], ex[s4], den[s4], ALU.mult)
                k.dma(wd[t * TT + s4 * 128:t * TT + (s4 + 1) * 128, :], wdt[s4], q="sp", is_output=True)
    return k.finish()

TWO_PI = 6.283185307179586
C1 = 6.28125
C2 = TWO_PI - C1


def build_post_ffn(Tc, moe, mla, gate_c, kv_cols=None, q_cols=None, TT=512):
    k = K()
    hT = k.dram("hT", [D, Tc], F32, "ExternalInput")
    mods = k.dram("mods", [128, 224], F32, "ExternalInput")
    gains = k.dram("gains", [128, 3, DC], F32, "ExternalInput")
    houtT = k.dram("houtT", [D, Tc], F32, "ExternalOutput")
    if moe:
        yaT = k.dram("yaT", [D, Tc], BF16, "ExternalInput")
        ybT = k.dram("ybT", [D, Tc], BF16, "ExternalInput")
        wab = k.dram("wab", [128, 2, Tc], F32, "ExternalInput")
    else:
        yT = k.dram("yT", [D, Tc], BF16, "ExternalInput")
    if mla:
        g4 = k.dram("g4", [128, 2, 4], F32, "ExternalInput")
        wkva = k.dram("wkva", [D, 576], F32, "ExternalInput")
        wkvb = k.dram("wkvb", [512, 4096], F32, "ExternalInput")
        wqa = k.dram("wqa", [D, 512], F32, "ExternalInput")
        wqb = k.dram("wqb", [512, 3072], F32, "ExternalInput")
        posd = k.dram("pos", [128, Tc], I32, "ExternalInput")
        invfd = k.dram("invf", [128, 1], F32, "ExternalInput")
        kvT = k.dram("kvT", [4096, Tc], BF16, "ExternalOutput")
        krT = k.dram("krT", [64, Tc], BF16, "ExternalOutput")
        qT = k.dram("qT", [3072, Tc], BF16, "ExternalOutput")
    cx = Ctx(k)
    m_sb = k.sb("m_sb", [128, 224], F32)
    g_sb = k.sb("g_sb", [128, 3, DC], F32)
    k.dma(m_sb, mods)
    k.dma(g_sb, gains)
    Gc = gate_coefs(k, "pf", m_sb, g_sb[:, 0, :], gate_c)
    h_sb = k.sb("h_sb", [128, DC, TT], F32, units=DC)
    sq = k.sb("sq", [128, DC, TT], BF16, units=DC)
    tmp = k.sb("tmp", [128, DC, TT], F32, units=DC)
    rs = k.sb("rs", [128, TT], F32)
    ps_n = k.ps("ps_n")
    if moe:
        ya_sb = k.sb("ya_sb", [128, DC, TT], BF16, units=DC)
        yb_sb = k.sb("yb_sb", [128, DC, TT], BF16, units=DC)
        y_sb = k.sb("y_sb", [128, DC, TT], F32, units=DC)
        w_sb = k.sb("w_sb", [128, 2, TT], F32)
    else:
        y_sb = k.sb("y_sb", [128, DC, TT], BF16, units=DC)
    if mla:
        g4_sb = k.sb("g4_sb", [128, 2, 4], F32)
        k.dma(g4_sb, g4)
        invf = k.sb("invf_sb", [128, 1], F32)
        k.dma(invf, invfd)
        Akv, Bkv = mod_coefs(k, "kv", m_sb, g_sb[:, 1, :], kv_cols[0], kv_cols[1])
        Aq, Bq = mod_coefs(k, "q", m_sb, g_sb[:, 2, :], q_cols[0], q_cols[1])
        un = k.sb("un", [128, DC, TT], BF16, units=DC)
        lat = k.sb("lat", [128, 4, TT], F32, units=4)
        latb = k.sb("latb", [128, 4, TT], BF16, units=4)
        kr12 = k.sb("kr12", [32, 2, TT], F32, units=2)
        qrp = k.sb("qrp", [128, 8, TT], F32, units=8)
        pos_i = k.sb("pos_i", [128, TT], I32)
        ang = k.sb("ang", [128, TT], F32)
        tq = k.sb("tq", [128, TT], F32)
        ki = k.sb("ki", [128, TT], I32)
        kf = k.sb("kf", [128, TT], F32)
        mk = k.sb("mk", [128, TT], F32)
        sin_t = k.sb("sin_t", [128, TT], F32)
        cos_t = k.sb("cos_t", [128, TT], F32)
        ra = k.sb("ra", [128, TT], F32)
        rb = k.sb("rb", [128, TT], F32)
        slabs = Slabs(k, "slw", DC, 3)
        slabs4 = Slabs(k, "slw4", 4, 3)
        psr = PsRot(k, 6)
        ost = [k.sb(f"ost{i}", [128, TT], BF16) for i in range(3)]
    n = 0
    for t in range(Tc // TT):
        ts_ = slice(t * TT, (t + 1) * TT)
        for c in range(DC):
            k.dma(h_sb[c], hT[c * 128:(c + 1) * 128, ts_], q=("sp" if c % 2 == 0 else "act"))
        if moe:
            k.dma(w_sb, wab[:, :, ts_])
            for c in range(DC):
                k.dma(ya_sb[c], yaT[c * 128:(c + 1) * 128, ts_], q="sp")
                k.dma(yb_sb[c], ybT[c * 128:(c + 1) * 128, ts_], q="act")
            for c in range(DC):
                k.tt(tmp[c], ya_sb[c], w_sb[:, 0, :], ALU.mult)
                k.tt(y_sb[c], yb_sb[c], w_sb[:, 1, :], ALU.mult)
                k.tt(y_sb[c], y_sb[c], tmp[c], ALU.add)
        else:
            for c in range(DC):
                k.dma(y_sb[c], yT[c * 128:(c + 1) * 128, ts_], q=("sp" if c % 2 == 1 else "act"))
        cx.rstd(k, y_sb, TT, sq, ps_n, rs, D)
        for c in range(DC):
            k.tt(tmp[c], y_sb[c], rs, ALU.mult)
            k.stt(h_sb[c], tmp[c], Gc[:, c:c + 1], h_sb[c], ALU.mult, ALU.add)
            k.dma(houtT[c * 128:(c + 1) * 128, ts_], h_sb[c], q="sp", is_output=True)
        if not mla:
            continue
        cx.rstd(k, h_sb, TT, sq, ps_n, rs, D)
        k.dma(pos_i, posd[:, ts_])
        k.copy(ang, pos_i)
        k.ts(ang, ang, invf[:, 0:1], ALU.mult)
        k.ts(tq, ang, 1.0 / TWO_PI, ALU.mult)
        k.copy(ki, tq)
        k.copy(kf, ki)
        k.stt(ang, kf, -C1, ang, ALU.mult, ALU.add)
        k.stt(ang, kf, -C2, ang, ALU.mult, ALU.add)
        k.ts(mk, ang, float(np.pi), ALU.is_gt)
        k.stt(ang, mk, -TWO_PI, ang, ALU.mult, ALU.add)
        k.ts(mk, ang, -float(np.pi), ALU.is_lt)
        k.stt(ang, mk, TWO_PI, ang, ALU.mult, ALU.add)
        k.act(sin_t, ang, AF.Sin)
        k.ts(tq, ang, float(np.pi / 2), ALU.add)
        k.ts(mk, tq, float(np.pi), ALU.is_gt)
        k.stt(tq, mk, -TWO_PI, tq, ALU.mult, ALU.add)
        k.act(cos_t, tq, AF.Sin)

        def rope(x1, x2, o1, o2, P):
            k.tt(ra[0:P], x1, cos_t[0:P], ALU.mult)
            k.tt(rb[0:P], x2, sin_t[0:P], ALU.mult)
            k.tt(o1, ra[0:P], rb[0:P], ALU.subtract)
            k.tt(ra[0:P], x2, cos_t[0:P], ALU.mult)
            k.tt(rb[0:P], x1, sin_t[0:P], ALU.mult)
            k.tt(o2, ra[0:P], rb[0:P], ALU.add)

        for c in range(DC):
            k.tt(tmp[c], h_sb[c], rs, ALU.mult)
            k.act(un[c], tmp[c], AF.Identity, bias=Bkv[:, c:c + 1], scale=Akv[:, c:c + 1])

        def cons_lat(j, ps):
            k.copy(lat[j], ps, eng=("act" if j % 2 else "dve"))
        linear(k, slabs, wkva, 0, 4, un, TT, psr, cons_lat)

        def cons_kr(j, ps):
            k.copy(kr12[j], ps[0:32, :], eng="act")
        linear(k, slabs, wkva, 512, 2, un, TT, psr, cons_kr, width=32)
        cx.rstd(k, lat, TT, sq, ps_n, rs2 := k_rs2(k), 512)
        for c in range(4):
            k.tt(lat[c], lat[c], rs2, ALU.mult)
            k.ts(latb[c], lat[c], g4_sb[:, 0, c:c + 1], ALU.mult)

        def cons_kv(j, ps):
            nonlocal n
            o = ost[n % 3]
            k.copy(o, ps, eng=("act" if n % 2 else "dve"))
            n += 1
            k.dma(kvT[j * 128:(j + 1) * 128, ts_], o, q="sp", is_output=True)
        linear(k, slabs4, wkvb, 0, 32, latb, TT, psr, cons_kv)
        o = ost[n % 3]
        n += 1
        rope(kr12[0], kr12[1], o[0:32], o[32:64], 32)
        k.dma(krT[:, ts_], o[0:64], q="sp", is_output=True)
        for c in range(DC):
            k.act(un[c], tmp[c], AF.Identity, bias=Bq[:, c:c + 1], scale=Aq[:, c:c + 1])
        linear(k, slabs, wqa, 0, 4, un, TT, psr, cons_lat)
        cx.rstd(k, lat, TT, sq, ps_n, rs2, 512)
        for c in range(4):
            k.tt(lat[c], lat[c], rs2, ALU.mult)
            k.ts(latb[c], lat[c], g4_sb[:, 1, c:c + 1], ALU.mult)

        def cons_q(j, ps):
            nonlocal n
            if j < 16:
                o = ost[n % 3]
                k.copy(o, ps, eng=("act" if n % 2 else "dve"))
                n += 1
                k.dma(qT[j * 128:(j + 1) * 128, ts_], o, q="sp", is_output=True)
            else:
                k.copy(qrp[j - 16], ps, eng=("act" if j % 2 else "dve"))
        linear(k, slabs4, wqb, 0, 24, latb, TT, psr, cons_q)
        for j in range(4):
            o1 = ost[n % 3]
            n += 1
            o2 = ost[n % 3]
            n += 1
            rope(qrp[j], qrp[4 + j], o1, o2, 128)
            k.dma(qT[2048 + j * 128:2048 + (j + 1) * 128, ts_], o1, q="sp", is_output=True)
            k.dma(qT[2560 + j * 128:2560 + (j + 1) * 128, ts_], o2, q="sp", is_output=True)
    return k.finish()


_rs2 = {}


def k_rs2(k):
    if id(k) not in _rs2:
        _rs2[id(k)] = k.sb("rs2", [128, 512], F32)
    return _rs2[id(k)]


import time as _time
import ml_dtypes
from concourse.bass_utils import run_bass_kernel_spmd

NCORES = 8
BF = ml_dtypes.bfloat16


def _fm(v):
    return np.ascontiguousarray(np.asarray(v, np.float32).reshape(-1, 128).T)


def _run(nc, in_maps, tag):
    t0 = _time.time()
    if os.environ.get("KPROF"):
        res = run_bass_kernel_spmd(nc, in_maps, core_ids=list(range(NCORES)), trace=True)
        print(f"[kernel] {tag}: exec_time_ns={res.exec_time_ns}", flush=True)
    else:
        res = run_bass_kernel_spmd(nc, in_maps, core_ids=list(range(NCORES)))
    print(f"[kernel] {tag}: {_time.time() - t0:.1f}s", flush=True)
    return res.results


def kernel(x, c, positions, ada_w, ada_b, norm_g, hg_w_in, hg_lb_logits, hg_out_norm_g,
           hg_w_out, kv_src_norm_g, kv_src_ada_w, kv_src_ada_b, mla_w_kv_a, mla_kv_norm_g,
           mla_w_kv_b, mla_w_q_a, mla_q_norm_g, mla_w_q_b, mla_w_o, ffn_w_gu, ffn_w_down,
           moe_w_router, moe_w_gu, moe_w_down):
    f32 = np.float32
    x = np.asarray(x, f32)
    Bn, S, _ = x.shape
    T = Bn * S
    Tc = T // NCORES
    cpb = NCORES // Bn
    A = lambda a: np.asarray(a)
    ada_w, ada_b, norm_g = A(ada_w), A(ada_b), A(norm_g)
    ident = np.eye(128, dtype=f32).astype(BF)

    W_all = np.concatenate([ada_w[0, 0], ada_w[0, 1], ada_w[1, 0], ada_w[1, 1], A(kv_src_ada_w)], axis=1)
    b_all = np.concatenate([ada_b[0, 0], ada_b[0, 1], ada_b[1, 0], ada_b[1, 1], A(kv_src_ada_b)])
    NCH = W_all.shape[1] // 128 // NCORES
    cT = np.ascontiguousarray(A(c).astype(f32).T.reshape(DC, 128, Bn).transpose(1, 0, 2))
    nc = build_L0(NCH)
    ims = [{"cT": cT, "W": np.ascontiguousarray(W_all[:, i * NCH * 128:(i + 1) * NCH * 128]),
            "bias": _fm(b_all[i * NCH * 128:(i + 1) * NCH * 128])} for i in range(NCORES)]
    r = _run(nc, ims, "L0 mods")
    del W_all
    mods_all = np.concatenate([r[i]["out"] for i in range(NCORES)], axis=1)
    mods_b = [np.ascontiguousarray(mods_all[:, :, b]) for b in range(Bn)]

    def tok(i):
        b = i // cpb
        s0 = (i % cpb) * Tc
        return b, s0

    xT = []
    for i in range(NCORES):
        b, s0 = tok(i)
        xT.append(np.ascontiguousarray(x[b, s0:s0 + Tc, :].T))

    nc = build_L1(Tc)
    ims = [{"xT": xT[i], "mods": mods_b[tok(i)[0]], "gains": _fm(norm_g[0, 0, 0])} for i in range(NCORES)]
    r = _run(nc, ims, "L1 norm0")
    u0T = np.concatenate([r[i]["uT"] for i in range(NCORES)], axis=1)

    w_in = A(hg_w_in)[0]
    lbl_all = A(hg_lb_logits).astype(f32)
    tri = np.triu(np.ones((64, 64), f32))
    rmask = np.ones((128, 512), f32)
    rmask[:, ::64] = 0
    nc = build_L2(Bn, S)
    ims = []
    for i in range(NCORES):
        hs = (2 * i, 2 * i + 1)
        blocks = []
        for base in (0, 2048, 4096, 6144):
            for h in hs:
                blocks.append(w_in[:, base + h * 128: base + (h + 1) * 128])
        lbl = np.stack([np.stack([lbl_all[0, h * 128:(h + 1) * 128], lbl_all[1, h * 128:(h + 1) * 128]], -1) for h in hs], 1)
        ims.append({"uT": u0T, "Wc": np.ascontiguousarray(np.concatenate(blocks, 1)), "lbl": np.ascontiguousarray(lbl.astype(f32)),
                    "ident": ident, "tri": tri, "rmask": rmask})
    r = _run(nc, ims, "L2 hgrn2")
    del u0T
    oT_all = np.concatenate([r[i]["oT"] for i in range(NCORES)], axis=0)
    sgT_all = np.concatenate([r[i]["sgT"] for i in range(NCORES)], axis=0)

    def cols(a, i):
        return np.ascontiguousarray(a[:, i * Tc:(i + 1) * Tc])

    nc = build_post_mixer(Tc, True, False, 32, 48, 64)
    g3 = np.ascontiguousarray(np.stack([_fm(A(hg_out_norm_g)[0]), _fm(norm_g[0, 0, 1]), _fm(norm_g[0, 1, 0])], 1))
    w_out = np.ascontiguousarray(A(hg_w_out)[0])
    ims = [{"hT": xT[i], "oT": cols(oT_all, i), "sgT": cols(sgT_all, i), "W": w_out, "mods": mods_b[tok(i)[0]], "gains": g3}
           for i in range(NCORES)]
    r = _run(nc, ims, "L3a post-hgrn2")
    del oT_all, sgT_all, xT
    h1T = [r[i]["houtT"] for i in range(NCORES)]
    u1T = [r[i]["uT"] for i in range(NCORES)]

    nc = build_ffn(Tc)
    wgu = np.ascontiguousarray(A(ffn_w_gu)[0])
    wdn = np.ascontiguousarray(A(ffn_w_down)[0])
    ims = [{"uT": u1T[i], "wgu": wgu, "wdn": wdn} for i in range(NCORES)]
    r = _run(nc, ims, "FFN dense")
    y2T = [r[i]["yT"] for i in range(NCORES)]
    del u1T, wgu, wdn

    nc = build_post_ffn(Tc, False, True, 80, kv_cols=(192, 208), q_cols=(96, 112))
    g3 = np.ascontiguousarray(np.stack([_fm(norm_g[0, 1, 1]), _fm(A(kv_src_norm_g)), _fm(norm_g[1, 0, 0])], 1))
    g4 = np.ascontiguousarray(np.stack([_fm(A(mla_kv_norm_g)), _fm(A(mla_q_norm_g)[0])], 1))
    perm = np.concatenate([[h * 192 + d for h in range(16) for d in range(128)],
                           [h * 192 + 128 + j for h in range(16) for j in range(32)],
                           [h * 192 + 160 + j for h in range(16) for j in range(32)]]).astype(np.int64)
    wqb = np.ascontiguousarray(A(mla_w_q_b)[0][:, perm])
    invf32 = (1.0 / (10000.0 ** (np.arange(0, 64, 2, dtype=f32) / 64))).astype(f32)
    invf = np.ascontiguousarray(np.concatenate([invf32] * 4).reshape(128, 1))
    pos = A(positions).astype(np.int32)
    ims = []
    for i in range(NCORES):
        b, s0 = tok(i)
        ims.append({"hT": h1T[i], "mods": mods_b[b], "gains": g3, "yT": y2T[i], "g4": g4,
                    "wkva": np.ascontiguousarray(A(mla_w_kv_a)), "wkvb": np.ascontiguousarray(A(mla_w_kv_b)),
                    "wqa": np.ascontiguousarray(A(mla_w_q_a)[0]), "wqb": wqb,
                    "pos": np.ascontiguousarray(np.broadcast_to(pos[b, s0:s0 + Tc][None], (128, Tc))), "invf": invf})
    r = _run(nc, ims, "L3c post-ffn + mla proj")
    del h1T, y2T
    h2T = [r[i]["houtT"] for i in range(NCORES)]
    kvT = np.concatenate([r[i]["kvT"] for i in range(NCORES)], axis=1)
    krT = np.ascontiguousarray(np.concatenate([r[i]["krT"] for i in range(NCORES)], axis=1))
    qT = np.concatenate([r[i]["qT"] for i in range(NCORES)], axis=1)

    kk = np.arange(128)[:, None, None]
    jj = np.arange(4)[None, :, None]
    qq = np.arange(512)[None, None, :]
    masks = ((kk + 128 * jj) <= qq).astype(f32).astype(BF)
    nc = build_L4(Bn, S)
    ims = []
    for i in range(NCORES):
        hs = (2 * i, 2 * i + 1)
        ims.append({
            "qn": np.ascontiguousarray(np.stack([qT[h * 128:(h + 1) * 128] for h in hs])),
            "qr": np.ascontiguousarray(np.stack([np.concatenate([qT[2048 + h * 32:2048 + (h + 1) * 32], qT[2560 + h * 32:2560 + (h + 1) * 32]], 0) for h in hs])),
            "kn": np.ascontiguousarray(np.stack([kvT[h * 256:h * 256 + 128] for h in hs])),
            "kr": krT,
            "v": np.ascontiguousarray(np.stack([kvT[h * 256 + 128:h * 256 + 256].T for h in hs])),
            "masks": masks, "ident": ident})
    r = _run(nc, ims, "L4 attention")
    del kvT, qT
    aT_all = np.concatenate([r[i]["oT"][hh].T for i in range(NCORES) for hh in range(2)], axis=0)

    nc = build_post_mixer(Tc, False, True, 128, 144, 160)
    g3 = np.ascontiguousarray(np.stack([_fm(np.ones(D, f32)), _fm(norm_g[1, 0, 1]), _fm(norm_g[1, 1, 0])], 1))
    w_o = np.ascontiguousarray(A(mla_w_o)[0])
    wr = np.ascontiguousarray(A(moe_w_router)[0].astype(f32))
    ims = [{"hT": h2T[i], "oT": cols(aT_all, i), "W": w_o, "mods": mods_b[tok(i)[0]], "gains": g3, "wr": wr} for i in range(NCORES)]
    r = _run(nc, ims, "L5 post-attn + router")
    del aT_all, h2T
    h3T = [r[i]["houtT"] for i in range(NCORES)]
    u3T = np.concatenate([r[i]["uT"] for i in range(NCORES)], axis=1)
    wd = np.concatenate([r[i]["wd"] for i in range(NCORES)], axis=0)

    sel = wd > 0
    NE = sel.shape[1]
    rank = np.cumsum(sel, axis=1)
    first = sel & (rank == 1)
    second = sel & (rank == 2)
    toks = [np.nonzero(first[:, e] | second[:, e])[0] for e in range(NE)]
    nmax = max(1, max(len(tk) for tk in toks))
    CAP = ((nmax + 511) // 512) * 512
    print(f"[kernel] expert loads {[len(tk) for tk in toks]} CAP={CAP}", flush=True)
    nc = build_ffn(CAP)
    mgu, mdn = A(moe_w_gu)[0], A(moe_w_down)[0]
    ims = []
    for e in range(NE):
        ug = np.zeros((D, CAP), BF)
        ug[:, :len(toks[e])] = u3T[:, toks[e]]
        ims.append({"uT": ug, "wgu": np.ascontiguousarray(mgu[e]), "wdn": np.ascontiguousarray(mdn[e])})
    r = _run(nc, ims, "FFN experts")
    del u3T
    yaT = np.zeros((D, T), BF)
    ybT = np.zeros((D, T), BF)
    wa = np.zeros(T, f32)
    wb = np.zeros(T, f32)
    for e in range(NE):
        tk = toks[e]
        ye = r[e]["yT"][:, :len(tk)]
        f1 = first[tk, e]
        yaT[:, tk[f1]] = ye[:, f1]
        ybT[:, tk[~f1]] = ye[:, ~f1]
        wa[tk[f1]] = wd[tk[f1], e]
        wb[tk[~f1]] = wd[tk[~f1], e]

    nc = build_post_ffn(Tc, True, False, 176)
    g3 = np.ascontiguousarray(np.stack([_fm(norm_g[1, 1, 1]), _fm(np.ones(D, f32)), _fm(np.ones(D, f32))], 1))
    ims = []
    for i in range(NCORES):
        sl = slice(i * Tc, (i + 1) * Tc)
        wab = np.ascontiguousarray(np.broadcast_to(np.stack([wa[sl], wb[sl]])[None], (128, 2, Tc)))
        ims.append({"hT": h3T[i], "mods": mods_b[tok(i)[0]], "gains": g3, "yaT": cols(yaT, i), "ybT": cols(ybT, i), "wab": wab})
    r = _run(nc, ims, "L7 moe combine")
    out = np.empty((Bn, S, D), f32)
    for i in range(NCORES):
        b, s0 = tok(i)
        out[b, s0:s0 + Tc, :] = r[i]["houtT"].T
    return out
```
